# Optimizing a Trainium2 kernel written in Bass

```python
import math
import jax
import jax.numpy as jnp
from jax import lax
import numpy as np

D_MODEL = 1024
BATCH = 16
SEQ = 2048
DEPTH = 1
DEC_BATCH = 2
DEC_SEQ = 16384
PAST_LEN = 128

MIX_WIDTH = D_MODEL
DN_HEADS = 4
DN_HEAD_DIM = 128
DN_WIDTH = DN_HEADS * DN_HEAD_DIM
ATT_HEADS = 8
ATT_HEAD_DIM = 64
ATT_WIDTH = ATT_HEADS * ATT_HEAD_DIM
CONV_K = 3
CHUNK = 64
DILATED_PATTERNS = ((128, 1), (512, 4), (2048, 16))
BAND = 64
NUM_BUCKETS = 32
MAX_DISTANCE = 1024
N_EXPERTS = 16
CAPACITY_FACTOR = 2
EXPERT_D_FF = 2816
EPS = 1e-6
PROJ_WIDTH = 4 * DN_WIDTH + 4 * DN_HEADS + 3 * ATT_WIDTH

kernel_name = 'hybrid_deltanet_dilated_attn_ec_moe_encoder'

F32 = jnp.float32


def rms_norm(x, g):
    xf = x.astype(F32)
    y = xf * lax.rsqrt(jnp.mean(xf * xf, axis=-1, keepdims=True) + EPS)
    return (y * g.astype(F32)).astype(x.dtype)


def l2norm(x):
    return x * lax.rsqrt(jnp.sum(x * x, axis=-1, keepdims=True) + EPS)


def centred_depthwise_conv(x, w):
    c = x.shape[-1]
    pad = (CONV_K - 1) // 2
    return lax.conv_general_dilated(
        x, w[:, None, :].astype(x.dtype), window_strides=(1,),
        padding=[(pad, CONV_K - 1 - pad)],
        dimension_numbers=('NWC', 'WIO', 'NWC'), feature_group_count=c)


def gated_delta_rule_chunked(q, k, v, g, beta):
    b, t, h, dk = q.shape
    dv = v.shape[-1]
    n = t // CHUNK

    def chunks(a):
        a = a.reshape(b, n, CHUNK, h, *a.shape[3:])
        return jnp.moveaxis(a, 3, 1)

    q = chunks(q * (dk ** -0.5))
    k = chunks(k)
    v = chunks(v)
    g = chunks(g)
    beta = chunks(beta)
    G = jnp.cumsum(g, axis=-1)
    tri = jnp.tril(jnp.ones((CHUNK, CHUNK), bool))
    strict = jnp.tril(jnp.ones((CHUNK, CHUNK), bool), -1)
    diff = G[..., :, None] - G[..., None, :]
    decay = jnp.where(tri, jnp.exp(jnp.where(tri, diff, 0.0)), 0.0)
    kbeta = k * beta[..., None]
    lower = jnp.where(strict, jnp.einsum('bhnid,bhnjd->bhnij', kbeta, k) * decay, 0.0)
    eye = jnp.eye(CHUNK, dtype=q.dtype)
    rhs = jnp.concatenate([v * beta[..., None], kbeta * jnp.exp(G)[..., None]], axis=-1)
    sol = lax.linalg.triangular_solve(lower + eye, rhs, left_side=True, lower=True,
                                      unit_diagonal=True)
    u, w = sol[..., :dv], sol[..., dv:]
    attn = jnp.where(tri, jnp.einsum('bhnid,bhnjd->bhnij', q, k) * decay, 0.0)
    q_dec = q * jnp.exp(G)[..., None]
    k_dec = k * jnp.exp(G[..., -1:] - G)[..., None]
    c_dec = jnp.exp(G[..., -1])
    xs = tuple(jnp.moveaxis(a, 2, 0) for a in (q_dec, k_dec, u, w, attn, c_dec))

    def step(state, inp):
        qd, kd, u_c, w_c, a_c, cd = inp
        v_new = u_c - jnp.einsum('bhcd,bhde->bhce', w_c, state)
        o = jnp.einsum('bhcd,bhde->bhce', qd, state) + jnp.einsum('bhij,bhje->bhie', a_c, v_new)
        state = state * cd[..., None, None] + jnp.einsum('bhcd,bhce->bhde', kd, v_new)
        return state, o

    state0 = jnp.zeros((b, h, dk, dv), q.dtype)
    _, o = lax.scan(step, state0, xs)
    return jnp.transpose(o, (1, 0, 3, 2, 4)).reshape(b, t, h, dv)


def gated_deltanet_mixer(qkv, z, b_raw, a_raw, conv_w, a_log_fwd, dt_bias_fwd,
                         a_log_bwd, dt_bias_bwd, dn_norm_g):
    bsz, s, _ = qkv.shape
    qkv = jax.nn.silu(centred_depthwise_conv(qkv, conv_w).astype(F32))
    q, k, v = jnp.split(qkv, 3, axis=-1)
    heads = lambda a: a.reshape(bsz, s, DN_HEADS, DN_HEAD_DIM)
    q = l2norm(heads(q))
    k = l2norm(heads(k))
    v = heads(v)
    b_raw = b_raw.astype(F32)
    a_raw = a_raw.astype(F32)
    beta_f = jax.nn.sigmoid(b_raw[..., :DN_HEADS])
    beta_b = jax.nn.sigmoid(b_raw[..., DN_HEADS:])
    g_f = -jnp.exp(a_log_fwd.astype(F32)) * jax.nn.softplus(a_raw[..., :DN_HEADS] + dt_bias_fwd.astype(F32))
    g_b = -jnp.exp(a_log_bwd.astype(F32)) * jax.nn.softplus(a_raw[..., DN_HEADS:] + dt_bias_bwd.astype(F32))
    flip = lambda a: jnp.flip(a, axis=1)
    o_f = gated_delta_rule_chunked(q, k, v, g_f, beta_f)
    o_b = flip(gated_delta_rule_chunked(flip(q), flip(k), flip(v), flip(g_b), flip(beta_b)))
    o = o_f + o_b
    o = o * lax.rsqrt(jnp.mean(o * o, axis=-1, keepdims=True) + EPS) * dn_norm_g.astype(F32)
    o = o * jax.nn.silu(heads(z.astype(F32)))
    return o.reshape(bsz, s, DN_WIDTH)


def t5_bucket(rel):
    nb = NUM_BUCKETS // 2
    ret = jnp.where(rel > 0, nb, 0)
    n = jnp.abs(rel)
    max_exact = nb // 2
    nf = jnp.maximum(n, 1).astype(F32)
    large = max_exact + (jnp.log(nf / max_exact) / math.log(MAX_DISTANCE / max_exact)
                         * (nb - max_exact)).astype(jnp.int32)
    large = jnp.minimum(large, nb - 1)
    return ret + jnp.where(n < max_exact, n, large)


def dilated_window_branch(q, k, v, rel_bias, window, dil):
    bsz, s, h, hd = q.shape
    L = s // dil
    half = window // (2 * dil)
    nb = -(-L // BAND)
    Lp = nb * BAND
    bp = bsz * dil

    def to_res(a):
        return a.reshape(bsz, L, dil, h, hd).transpose(0, 2, 1, 3, 4).reshape(bp, L, h, hd)

    qr, kr, vr = to_res(q), to_res(k), to_res(v)
    qb = jnp.pad(qr, ((0, 0), (0, Lp - L), (0, 0), (0, 0))).reshape(bp, nb, BAND, h, hd)
    kv_pad = ((0, 0), (BAND, Lp - L + BAND), (0, 0), (0, 0))

    def key_blocks(a):
        ab = jnp.pad(a, kv_pad).reshape(bp, nb + 2, BAND, h, hd)
        return jnp.concatenate([ab[:, :-2], ab[:, 1:-1], ab[:, 2:]], axis=2)

    kb, vb = key_blocks(kr), key_blocks(vr)
    logits = jnp.einsum('bnqhd,bnkhd->bnhqk', qb, kb) * (ATT_HEAD_DIM ** -0.5)
    q_off = jnp.arange(BAND)
    k_off = jnp.arange(3 * BAND) - BAND
    delta = k_off[None, :] - q_off[:, None]
    bias = rel_bias.astype(F32)[t5_bucket(delta * dil)].transpose(2, 0, 1)
    key_pos = jnp.arange(nb)[:, None] * BAND + k_off[None, :]
    mask = (jnp.abs(delta) <= half)[None] & ((key_pos >= 0) & (key_pos < L))[:, None, :]
    logits = jnp.where(mask[None, :, None], logits + bias[None, None], -jnp.inf)
    m = jnp.max(logits, axis=-1, keepdims=True)
    p = jnp.exp(logits - m)
    den = jnp.sum(p, axis=-1, keepdims=True)
    o = jnp.einsum('bnhqk,bnkhd->bnqhd', p / den, vb)
    lse = (m + jnp.log(den))[..., 0].transpose(0, 1, 3, 2)

    def from_res(a):
        rest = a.shape[3:]
        a = a.reshape(bp, Lp, *rest)[:, :L]
        return a.reshape(bsz, dil, L, *rest).swapaxes(1, 2).reshape(bsz, s, *rest)

    return from_res(o), from_res(lse)


def dilated_mixture_attention(q, k, v, rel_bias):
    outs, lses = [], []
    for window, dil in DILATED_PATTERNS:
        o, lse = dilated_window_branch(q, k, v, rel_bias, window, dil)
        outs.append(o)
        lses.append(lse)
    wts = jax.nn.softmax(jnp.stack(lses, axis=0), axis=0)
    return jnp.sum(wts[..., None] * jnp.stack(outs, axis=0), axis=0)


def expert_choice_ffn(h, w_router, w_gate, w_up, w_down):
    bsz, s, d = h.shape
    n_tok = bsz * s
    cap = CAPACITY_FACTOR * n_tok // N_EXPERTS
    hf = h.reshape(n_tok, d)
    aff = jax.nn.softmax(jnp.einsum('nd,de->ne', hf.astype(F32), w_router.astype(F32)), axis=-1)
    gate, idx = lax.top_k(aff.T, cap)
    xe = hf[idx]
    a = jnp.einsum('ecd,edf->ecf', xe, w_gate)
    b = jnp.einsum('ecd,edf->ecf', xe, w_up)
    ye = jnp.einsum('ecf,efd->ecd', jax.nn.silu(a) * b, w_down)
    contrib = (gate[..., None].astype(ye.dtype) * ye).reshape(-1, d)
    out = jnp.zeros((n_tok, d), ye.dtype).at[idx.reshape(-1)].add(contrib)
    return out.reshape(bsz, s, d)


def encoder_layer(x, rel_bias, norm1_g, w_in, conv_w, a_log_fwd, dt_bias_fwd, a_log_bwd,
                  dt_bias_bwd, dn_norm_g, w_out, norm2_g, w_router, w_gate, w_up, w_down):
    bsz, s, _ = x.shape
    h = rms_norm(x, norm1_g)
    proj = jnp.einsum('bsd,dp->bsp', h, w_in)
    o_qkv = 3 * DN_WIDTH
    o_z = o_qkv + DN_WIDTH
    o_beta = o_z + 2 * DN_HEADS
    o_a = o_beta + 2 * DN_HEADS
    dn_out = gated_deltanet_mixer(proj[..., :o_qkv], proj[..., o_qkv:o_z], proj[..., o_z:o_beta],
                                  proj[..., o_beta:o_a], conv_w, a_log_fwd, dt_bias_fwd,
                                  a_log_bwd, dt_bias_bwd, dn_norm_g)
    att_q, att_k, att_v = jnp.split(proj[..., o_a:].astype(F32), 3, axis=-1)
    heads = lambda a: a.reshape(bsz, s, ATT_HEADS, ATT_HEAD_DIM)
    att_out = dilated_mixture_attention(heads(att_q), heads(att_k), heads(att_v), rel_bias)
    mixed = jnp.concatenate([dn_out, att_out.reshape(bsz, s, ATT_WIDTH)], axis=-1).astype(x.dtype)
    x = x + jnp.einsum('bsm,md->bsd', mixed, w_out)
    x = x + expert_choice_ffn(rms_norm(x, norm2_g), w_router, w_gate, w_up, w_down).astype(x.dtype)
    return x


def setup_inputs(seed: int = 0) -> dict:
    key = jax.random.key(seed)
    ks = jax.random.split(key, 20)
    nrm = lambda k, shape, scale: jax.random.normal(k, shape, F32) * scale
    return {
        'x_prompt': nrm(ks[0], (BATCH, SEQ, D_MODEL), 1.0),
        'x_sample': nrm(ks[1], (DEC_BATCH, DEC_SEQ, D_MODEL), 1.0),
        'rel_bias': nrm(ks[2], (NUM_BUCKETS, ATT_HEADS), 0.5),
        'norm1_g': 1.0 + nrm(ks[3], (DEPTH, D_MODEL), 0.02),
        'w_in': nrm(ks[4], (DEPTH, D_MODEL, PROJ_WIDTH), D_MODEL ** -0.5),
        'conv_w': nrm(ks[5], (DEPTH, CONV_K, 3 * DN_WIDTH), CONV_K ** -0.5),
        'a_log_fwd': jnp.log(jax.random.uniform(ks[6], (DEPTH, DN_HEADS), F32, 1.0, 16.0)),
        'dt_bias_fwd': 1.0 + nrm(ks[7], (DEPTH, DN_HEADS), 0.1),
        'a_log_bwd': jnp.log(jax.random.uniform(ks[8], (DEPTH, DN_HEADS), F32, 1.0, 16.0)),
        'dt_bias_bwd': 1.0 + nrm(ks[9], (DEPTH, DN_HEADS), 0.1),
        'dn_norm_g': 1.0 + nrm(ks[10], (DEPTH, DN_HEAD_DIM), 0.02),
        'w_out': nrm(ks[11], (DEPTH, MIX_WIDTH, D_MODEL), MIX_WIDTH ** -0.5),
        'norm2_g': 1.0 + nrm(ks[12], (DEPTH, D_MODEL), 0.02),
        'w_router': nrm(ks[13], (DEPTH, D_MODEL, N_EXPERTS), D_MODEL ** -0.5),
        'w_gate': nrm(ks[14], (DEPTH, N_EXPERTS, D_MODEL, EXPERT_D_FF), D_MODEL ** -0.5),
        'w_up': nrm(ks[15], (DEPTH, N_EXPERTS, D_MODEL, EXPERT_D_FF), D_MODEL ** -0.5),
        'w_down': nrm(ks[16], (DEPTH, N_EXPERTS, EXPERT_D_FF, D_MODEL), EXPERT_D_FF ** -0.5),
        'final_norm_g': 1.0 + nrm(ks[17], (D_MODEL,), 0.02),
    }


def reference(x_prompt, x_sample, rel_bias, norm1_g, w_in, conv_w, a_log_fwd, dt_bias_fwd,
              a_log_bwd, dt_bias_bwd, dn_norm_g, w_out, norm2_g, w_router, w_gate, w_up,
              w_down, final_norm_g):
    def trunk(x):
        for l in range(DEPTH):
            x = encoder_layer(x, rel_bias, norm1_g[l], w_in[l], conv_w[l], a_log_fwd[l],
                              dt_bias_fwd[l], a_log_bwd[l], dt_bias_bwd[l], dn_norm_g[l],
                              w_out[l], norm2_g[l], w_router[l], w_gate[l], w_up[l], w_down[l])
        return rms_norm(x, final_norm_g)

    y_prompt = trunk(x_prompt)
    y_sample = trunk(x_sample)
    return (y_prompt, y_sample)
```

```python
import contextlib
import numpy as np
import ml_dtypes
import concourse.bass as bass
import concourse.mybir as mybir
from concourse.bass_utils import run_bass_kernel_spmd

F32 = mybir.dt.float32
BF16 = mybir.dt.bfloat16
I32 = mybir.dt.int32
U32 = mybir.dt.uint32
AF = mybir.ActivationFunctionType
ALU = mybir.AluOpType
AX = mybir.AxisListType

H = 1024
D = 1024
PW = 3600
EPS = 1e-6
NCORES = 8


class KB:
    ND = 12

    def __init__(self, nc):
        self.nc = nc
        self.E = {'pe': nc.tensor, 'dve': nc.vector, 'act': nc.scalar,
                  'pool': nc.gpsimd, 'sp': nc.sync}
        self.sems = {}
        self.cnt = {}
        for e in self.E:
            self.sems[e] = nc.semaphore('s_' + e).__enter__()
            self.cnt[e] = 0
        self.seen = {e: {} for e in self.E}
        self.dcnt = {}
        self.dnext = {}
        for q in ('sp', 'act', 'pool'):
            self.dnext[q] = 0
            for i in range(self.ND):
                k = ('d', q, i)
                self.sems[k] = nc.semaphore('d_%s_%d' % (q, i)).__enter__()
                self.dcnt[k] = 0
        self.lastw = {}
        self.readers = {}
        self.ninst = 0

    def wait(self, eng, tok):
        k, v = tok
        if self.seen[eng].get(k, 0) >= v:
            return
        if k == eng and eng == 'pe':
            return
        self.E[eng].wait_ge(self.sems[k], v)
        self.seen[eng][k] = v
        self.ninst += 1

    def uq(self):
        self._u = getattr(self, '_u', 0) + 1
        return ('uq', self._u)

    def _deps(self, eng, reads, writes, war=()):
        for b in war:
            for t in list(self.readers.get(b, {}).items()):
                self.wait(eng, t)
        for b in list(reads) + list(writes):
            t = self.lastw.get(b)
            if t is not None:
                self.wait(eng, t)
        for b in writes:
            for t in list(self.readers.get(b, {}).items()):
                self.wait(eng, t)

    def _commit(self, tok, reads, writes):
        for b in writes:
            self.lastw[b] = tok
            self.readers[b] = {}
        for b in reads:
            r = self.readers.setdefault(b, {})
            if r.get(tok[0], 0) < tok[1]:
                r[tok[0]] = tok[1]

    def op(self, eng, fn, reads=(), writes=()):
        self._deps(eng, reads, writes)
        inst = fn(self.E[eng])
        self.cnt[eng] += 1
        inst.then_inc(self.sems[eng], 1)
        tok = (eng, self.cnt[eng])
        self.ninst += 1
        self._commit(tok, reads, writes)
        return tok

    def _dma_like(self, q, fn, reads, writes, war=()):
        slot = self.dnext[q]
        self.dnext[q] = (slot + 1) % self.ND
        k = ('d', q, slot)
        if self.dcnt[k] > 0:
            self.wait(q, (k, 16 * self.dcnt[k]))
        self._deps(q, reads, writes, war)
        inst = fn(self.E[q])
        self.dcnt[k] += 1
        inst.then_inc(self.sems[k], 16)
        tok = (k, 16 * self.dcnt[k])
        self.ninst += 1
        self._commit(tok, reads, writes)
        return tok

    def dma(self, q, out, in_, reads=(), writes=(), war=(), **kw):
        return self._dma_like(q, lambda e: e.dma_start(out=out, in_=in_, **kw), reads, writes, war)

    def barrier(self):
        for eng in self.E:
            for k, c in self.dcnt.items():
                if c > 0:
                    self.wait(eng, (k, 16 * c))
            for e in self.E:
                if self.cnt[e] > 0:
                    self.wait(eng, (e, self.cnt[e]))
        self.lastw = {}
        self.readers = {}


class Rot:
    def __init__(self, items):
        self.items = items
        self.i = 0

    def next(self):
        t = self.items[self.i % len(self.items)]
        self.i += 1
        return t


class Cfg:
    def __init__(self, segs):
        self.segs = segs
        self.slot_base = []
        self.act_base = []
        b = a = 0
        for body, full in segs:
            self.slot_base.append(b)
            self.act_base.append(a)
            b += body + 2 * H
            a += (body + 2 * H) if full else body
        self.NTS = b
        self.NBODY = sum(bd for bd, _ in segs)
        self.NACT = a


FULL_CFG = Cfg([(2048, False), (2048, False), (4096, True)])


def phase1a(kb, cfg, io, scr):
    nc = kb.nc
    with contextlib.ExitStack() as es:
        def sb(name, shape, dt):
            return es.enter_context(nc.sbuf_tensor(name, list(shape), dt))

        def ps(name, shape, dt=F32):
            return es.enter_context(nc.psum_tensor(name, list(shape), dt))

        w_sb = sb("w_sb", [128, 8, PW], BF16)
        wst = [sb("wst%d" % i, [128, PW], F32) for i in range(2)]
        g_sb = sb("g1_sb", [128, 8], F32)
        ident = sb("ident", [128, 128], BF16)
        zero_t = sb("zero_t", [128, 4096], BF16)
        xts = Rot([sb("xt%d" % i, [128, D], F32) for i in range(3)])
        xns = Rot([sb("xn%d" % i, [128, D], BF16) for i in range(2)])
        junk = sb("junk", [128, D], BF16)
        sss = Rot([sb("ss%d" % i, [128, 4], F32) for i in range(3)])
        vals = Rot([sb("val%d" % i, [128, 1], F32) for i in range(8)])
        hTs = Rot([sb("hT%d" % i, [128, 8, 512], BF16) for i in range(2)])
        st32 = Rot([sb("st32_%d" % i, [128, 512], F32) for i in range(4)])
        st16 = Rot([sb("st16_%d" % i, [128, 512], BF16) for i in range(3)])
        stv = Rot([sb("stv%d" % i, [128, 4, 2, 128], BF16) for i in range(2)])
        stsc = Rot([sb("stsc%d" % i, [128, 16], F32) for i in range(2)])
        tps = Rot([ps("tp%d" % i, [128, 8, 128], BF16) for i in range(2)])
        pps = Rot([ps("pp%d" % i, [128, 512], F32) for i in range(4)])

        kb.dma('sp', ident[:], io['ident_bf'][:, :], writes=['ident'])
        kb.dma('sp', g_sb[:], io['g1'][:, :], writes=['g1'])
        kb.op('pool', lambda e: e.memset(zero_t[:], 0.0), writes=['zero_t'])
        for k in range(8):
            w = wst[k % 2]
            kb.dma('sp', w[:], io['w_in'][k * 128:(k + 1) * 128, :], writes=[w.name])
            kb.op('dve' if k % 2 == 0 else 'pool',
                  lambda e, w=w, k=k: e.tensor_scalar(out=w_sb[:, k, :], in0=w[:], scalar1=g_sb[:, k:k + 1],
                                                      scalar2=None, op0=ALU.mult),
                  reads=[w.name, 'g1'], writes=[('w_sb', k)])
        for s, (body, full) in enumerate(cfg.segs):
            if full:
                continue
            for side in range(0 if 'zero' in getattr(cfg, 'p1a_skip', ()) else 2):
                s0 = cfg.slot_base[s] + (0 if side == 0 else H + body)
                for r in range(4):
                    kb.dma('pool', scr['qkT'][512 + r * 128:512 + (r + 1) * 128, s0:s0 + H], zero_t[:, 0:H],
                           reads=['zero_t'], writes=[kb.uq()])
                for hh in range(2):
                    kb.dma('pool', scr['vaug'][s0 + hh * 512:s0 + (hh + 1) * 512, :].rearrange("(p j) c -> p j c", j=4),
                           zero_t[:].rearrange("p (j c) -> p j c", j=4), reads=['zero_t'], writes=[kb.uq()])

        evi = [0]

        def evac(out_ap, in_ap, reads, writes, eng=None):
            if eng is None:
                eng = 'act' if evi[0] % 2 == 0 else 'dve'
                evi[0] += 1
            if eng == 'act':
                return kb.op('act', lambda e: e.copy(out=out_ap, in_=in_ap), reads=reads, writes=writes)
            return kb.op('dve', lambda e: e.tensor_copy(out=out_ap, in_=in_ap), reads=reads, writes=writes)

        wkeys = [('w_sb', k) for k in range(8)]
        for s, (body, full) in enumerate(cfg.segs):
            nact = (body + 2 * H) if full else body
            for g in range(nact // 512):
                a0 = cfg.act_base[s] + g * 512
                s0 = cfg.slot_base[s] + (0 if full else H) + g * 512
                hT = hTs.next()
                vtiles = []
                for j in range(4):
                    xt = xts.next()
                    xn = xns.next()
                    ss = sss.next()
                    val = vals.next()
                    tp = tps.next()
                    vtiles.append(val)
                    r0 = a0 + j * 128
                    kb.dma('sp', xt[:], io['xs'][r0:r0 + 128, :], writes=[xt.name])
                    kb.dma('sp', val[:], io['valid'][r0:r0 + 128, :], writes=[val.name])
                    kb.op('act', lambda e: e.activation(out=junk[:], in_=xt[:], func=AF.Square,
                                                        accum_out=ss[:, 0:1]),
                          reads=[xt.name], writes=['junk', ss.name])
                    kb.op('act', lambda e: e.activation(out=ss[:, 1:2], in_=ss[:, 0:1], func=AF.Sqrt,
                                                        scale=1.0 / D, bias=EPS),
                          reads=[ss.name], writes=[ss.name])
                    kb.op('dve', lambda e: e.reciprocal(out=ss[:, 2:3], in_=ss[:, 1:2]),
                          reads=[ss.name], writes=[ss.name])
                    kb.op('dve', lambda e: e.tensor_scalar(out=xn[:], in0=xt[:], scalar1=ss[:, 2:3],
                                                           scalar2=None, op0=ALU.mult),
                          reads=[xt.name, ss.name], writes=[xn.name])
                    for k in range(8):
                        kb.op('pe', lambda e, k=k: e.transpose(out=tp[:, k, :], in_=xn[:, k * 128:(k + 1) * 128],
                                                               identity=ident[:]),
                              reads=[xn.name, 'ident'], writes=[tp.name])
                    evac(hT[:, :, j * 128:(j + 1) * 128], tp[:], [tp.name], [(hT.name, j)])
                hkeys = [(hT.name, j) for j in range(4)]
                fm_tiles = [(c * 128, 'dn', c) for c in range(12)] + [(2064 + c * 128, 'qk', c) for c in range(8)]
                g_lo, g_hi = g * 512, (g + 1) * 512
                need_dn = (not full) or (g_hi > H - 256 - 1 and g_lo < H + body + 256 + 1)
                need_body = (not full) or (g_hi > H and g_lo < H + body)
                if not need_dn:
                    fm_tiles = [t_ for t_ in fm_tiles if t_[1] != 'dn']
                if not need_body:
                    fm_tiles = [t_ for t_ in fm_tiles if not (t_[1] == 'qk' and t_[2] < 4)]
                for c0, kind, ci in fm_tiles:
                    pp = pps.next()
                    for k in range(8):
                        kb.op('pe', lambda e, k=k: e.matmul(pp[:, :], lhsT=w_sb[:, k, c0:c0 + 128], rhs=hT[:, k, :],
                                                            start=(k == 0), stop=(k == 7)),
                              reads=hkeys + [wkeys[k]], writes=[pp.name])
                    if kind == 'dn':
                        st = st32.next()
                        evac(st[:], pp[:], [pp.name], [st.name])
                        kb.dma('pool', scr['dnT'][ci * 128:(ci + 1) * 128, s0:s0 + 512], st[:],
                               reads=[st.name], writes=[kb.uq()])
                    else:
                        st = st16.next()
                        evac(st[:], pp[:], [pp.name], [st.name])
                        kb.dma('pool', scr['qkT'][ci * 128:(ci + 1) * 128, s0:s0 + 512], st[:],
                               reads=[st.name], writes=[kb.uq()])
                for j in range(4):
                    t0 = s0 + j * 128
                    if need_body:
                        pp = pps.next()
                        for k in range(8):
                            kb.op('pe', lambda e, k=k: e.matmul(pp[:, :], lhsT=hT[:, k, j * 128:(j + 1) * 128],
                                                                rhs=w_sb[:, k, 1536:2048], start=(k == 0), stop=(k == 7)),
                                  reads=hkeys + [wkeys[k]], writes=[pp.name])
                        st = st32.next()
                        evac(st[:], pp[:], [pp.name], [st.name])
                        kb.dma('pool', scr['zs'][t0:t0 + 128, :], st[:], reads=[st.name], writes=[kb.uq()])
                    if need_dn:
                        pp = pps.next()
                        for k in range(8):
                            kb.op('pe', lambda e, k=k: e.matmul(pp[:, 0:16], lhsT=hT[:, k, j * 128:(j + 1) * 128],
                                                                rhs=w_sb[:, k, 2048:2064], start=(k == 0), stop=(k == 7)),
                                  reads=hkeys + [wkeys[k]], writes=[pp.name])
                        stc = stsc.next()
                        evac(stc[:], pp[:, 0:16], [pp.name], [stc.name])
                        kb.dma('pool', scr['sc'][t0:t0 + 128, :], stc[:], reads=[stc.name], writes=[kb.uq()])
                    if 'v' in getattr(cfg, 'p1a_skip', ()):
                        continue
                    pp = pps.next()
                    for k in range(8):
                        kb.op('pe', lambda e, k=k: e.matmul(pp[:, :], lhsT=hT[:, k, j * 128:(j + 1) * 128],
                                                            rhs=w_sb[:, k, 3088:3600], start=(k == 0), stop=(k == 7)),
                              reads=hkeys + [wkeys[k]], writes=[pp.name])
                    sv = stv.next()
                    val = vtiles[j]
                    ppv = pp[:].rearrange("p (h t c) -> p h t c", h=4, t=2)
                    evac(sv[:, :, 0, 0:64], ppv[:, :, 0, :], [pp.name], [(sv.name, 0)], eng=getattr(cfg, 'p1a_ev', 'dve'))
                    evac(sv[:, :, 1, 64:128], ppv[:, :, 1, :], [pp.name], [(sv.name, 1)], eng=getattr(cfg, 'p1a_ev', 'dve'))
                    veng = getattr(cfg, 'p1a_veng', 'pool')
                    if veng != 'none':
                        kb.op(veng, lambda e: e.tensor_copy(out=sv[:, :, 0, 64:128],
                                                            in_=val[:, 0:1].unsqueeze(1).to_broadcast([128, 4, 64])),
                              reads=[val.name], writes=[(sv.name, 2)])
                        kb.op(veng, lambda e: e.tensor_copy(out=sv[:, :, 1, 0:64],
                                                            in_=val[:, 0:1].unsqueeze(1).to_broadcast([128, 4, 64])),
                              reads=[val.name], writes=[(sv.name, 3)])
                    kb.dma('pool', scr['vaug'][t0:t0 + 128, :], sv[:].rearrange("p h t c -> p (h t c)"),
                           reads=[(sv.name, i) for i in range(4)], writes=[kb.uq()])
        kb.barrier()


DILS = (1, 4, 16)


def sl_(start, n, step):
    return slice(start, start + (n - 1) * step + 1, step)


def body_base(cfg, s):
    return sum(b for b, _ in cfg.segs[:s])


def phase1c(kb, cfg, io, scr):
    nc = kb.nc
    maxbody = max(b for b, _ in cfg.segs)
    maxT = maxbody + 2 * H
    with contextlib.ExitStack() as es:
        def sb(name, shape, dt):
            return es.enter_context(nc.sbuf_tensor(name, list(shape), dt))

        def ps(name, shape, dt=F32):
            return es.enter_context(nc.psum_tensor(name, list(shape), dt))

        biasT = sb("biasT_sb", [128, 24, 256], F32)
        qTh = [sb("qTA", [128, maxbody], BF16), sb("qTB", [128, maxbody], BF16)]
        kT = sb("kT", [128, maxT], BF16)
        acc = [sb("accA", [128, maxbody + 16], F32), sb("accB", [128, maxbody + 16], F32)]
        ntile_max = maxbody // 128 + 16
        Vbs = Rot([sb("Vb%d" % i, [128, ntile_max, 256], BF16) for i in range(2)])
        tmps = Rot([sb("atmp%d" % i, [128, 512], F32) for i in range(4)])
        pTs = Rot([sb("apT%d" % i, [128, 512], BF16) for i in range(4)])
        rdens = Rot([sb("rden%d" % i, [128, 512], F32) for i in range(2)])
        mts = Rot([sb("mt%d" % i, [128, 512], BF16) for i in range(2)])
        sps = Rot([ps("sp%d" % i, [128, 512], F32) for i in range(4)])
        pos = Rot([ps("po%d" % i, [128, 512], F32) for i in range(2)])

        kb.dma('sp', biasT[:], io['biasT'][:, :, :], writes=['biasT'])
        kb.op('pool', lambda e: e.memset(qTh[0][64:128, :], 0.0), writes=[('qT', 0)])
        kb.op('pool', lambda e: e.memset(qTh[1][0:64, :], 0.0), writes=[('qT', 1)])
        for s, (body, full) in enumerate(cfg.segs):
            T = body + 2 * H
            sl0 = cfg.slot_base[s]
            bb = body_base(cfg, s)
            for hp in range(4):
                for hh in range(2):
                    kb.dma('sp', qTh[hh][hh * 64:(hh + 1) * 64, 0:body],
                           scr['qkT'][hp * 128 + hh * 64:hp * 128 + (hh + 1) * 64, sl0 + H:sl0 + H + body],
                           reads=[('qkT', s)], writes=[('qT', hh)])
                kb.dma('sp', kT[:, 0:T], scr['qkT'][512 + hp * 128:512 + (hp + 1) * 128, sl0:sl0 + T],
                       reads=[('qkT', s)], writes=['kT'])
                kb.op('pool', lambda e: e.memset(acc[0][:, 0:body], 0.0), writes=['accA'])
                kb.op('pool', lambda e: e.memset(acc[1][:, 0:body], 0.0), writes=['accB'])
                for b, d in enumerate(DILS):
                    if b not in getattr(cfg, 'att_branches', (0, 1, 2)):
                        continue
                    Vb = Vbs.next()
                    ntr = body // (128 * d) + 1
                    nbr = body // (128 * d)
                    for r in range(d):
                        start = sl0 + r + H - 64 * d
                        for m0 in range(0, ntr, 4):
                            mc = min(4, ntr - m0)
                            src = scr['vaug'][sl_(start + m0 * 128 * d, 128 * mc, d), hp * 256:(hp + 1) * 256]
                            kb.dma('sp', Vb[:, r * ntr + m0:r * ntr + m0 + mc, :],
                                   src.rearrange("(m j) c -> j m c", j=128),
                                   reads=[('vaug', s)], writes=[(Vb.name, r, m0)], war=[Vb.name])
                    if d == 1:
                        groups = [[(0, nb0 + i) for i in range(4)] for nb0 in range(0, nbr, 4)]
                    else:
                        groups = [[(r0 + i, nb) for i in range(4)] for nb in range(nbr) for r0 in range(0, d, 4)]
                    for h in range(2):
                        hs = slice(h * 64, (h + 1) * 64)
                        accn = 'accA' if h == 0 else 'accB'
                        bh = b * 8 + hp * 2 + h
                        tasks = []
                        for gi, grp in enumerate(groups):
                            tasks.append((gi, 0, grp[0:2]))
                            tasks.append((gi, 1, grp[2:4]))
                        spt = {}
                        pot = {}

                        def emit_S(ti):
                            gi, p, qbs = tasks[ti]
                            sp_ = sps.next()
                            spt[ti] = sp_
                            spv = sp_[:].rearrange("p (q k i) -> p q k i", q=2, k=2)
                            for q, (r, nb) in enumerate(qbs):
                                qc = r + d * nb * 128
                                for kt in range(2):
                                    kc = r + H - 64 * d + d * (nb + kt) * 128
                                    kb.op('pe', lambda e: e.matmul(spv[:, q, kt, :],
                                                                   lhsT=kT[:, sl_(kc, 128, d)],
                                                                   rhs=qTh[h][:, sl_(qc, 128, d)],
                                                                   start=True, stop=True),
                                          reads=[('qT', h), 'kT'], writes=[sp_.name])

                        def emit_rest(ti):
                            gi, p, qbs = tasks[ti]
                            sp_ = spt.pop(ti)
                            tmp = tmps.next()
                            pT = pTs.next()
                            kb.op('dve', lambda e: e.scalar_tensor_tensor(
                                out=tmp[:].rearrange("p (q c) -> p q c", q=2),
                                in0=sp_[:].rearrange("p (q c) -> p q c", q=2), scalar=0.125,
                                in1=biasT[:, bh:bh + 1, :].to_broadcast([128, 2, 256]),
                                op0=ALU.mult, op1=ALU.add),
                                reads=[sp_.name, 'biasT'], writes=[tmp.name])
                            kb.op('act', lambda e: e.activation(out=pT[:], in_=tmp[:], func=AF.Exp),
                                  reads=[tmp.name], writes=[pT.name])
                            if getattr(cfg, 'att_stage', 9) < 3:
                                return
                            if p == 0:
                                pot[gi] = pos.next()
                            po = pot[gi]
                            pov = po[:].rearrange("p (q i) -> p q i", q=4)
                            pTv = pT[:].rearrange("p (q k i) -> p q k i", q=2, k=2)
                            for q, (r, nb) in enumerate(qbs):
                                for kt in range(2):
                                    kb.op('pe', lambda e: e.matmul(pov[:, p * 2 + q, :],
                                                                   lhsT=Vb[:, r * ntr + nb + kt, h * 128:(h + 1) * 128],
                                                                   rhs=pTv[:, q, kt, :],
                                                                   start=(kt == 0), stop=(kt == 1)),
                                          reads=[pT.name, Vb.name, (Vb.name, r, ((nb + kt) // 4) * 4)], writes=[po.name])
                            if p == 1:
                                grp = groups[gi]
                                r0, nb0 = grp[0]
                                a = acc[h]
                                if d == 1:
                                    c0 = nb0 * 128
                                    av = a[:, c0:c0 + 512].rearrange("p (q i) -> p q i", q=4)
                                else:
                                    c0 = r0 + d * nb0 * 128
                                    av = a[:, c0:c0 + d * 128].rearrange("p (i dd) -> p dd i", dd=d)[:, 0:4, :]
                                kb.op('dve', lambda e: e.tensor_tensor(out=av, in0=av, in1=pov[:, :, :], op=ALU.add),
                                      reads=[po.name, accn], writes=[accn])
                                pot.pop(gi)

                        stage = getattr(cfg, 'att_stage', 9)
                        if stage >= 2:
                            emit_S(0)
                            if len(tasks) > 1:
                                emit_S(1)
                            for ti in range(len(tasks)):
                                if ti + 2 < len(tasks):
                                    emit_S(ti + 2)
                                emit_rest(ti)
                for c in range(body // 512 if getattr(cfg, 'att_stage', 9) >= 1 else 0):
                    cs = slice(c * 512, (c + 1) * 512)
                    rd = rdens.next()
                    mt = mts.next()
                    kb.op('dve', lambda e: e.reciprocal(out=rd[0:64, :], in_=acc[0][64:128, cs]),
                          reads=['accA'], writes=[(rd.name, 0)])
                    kb.op('dve', lambda e: e.reciprocal(out=rd[64:128, :], in_=acc[1][0:64, cs]),
                          reads=['accB'], writes=[(rd.name, 1)])
                    kb.op('pool', lambda e: e.tensor_tensor(out=mt[0:64, :], in0=acc[0][0:64, cs], in1=rd[0:64, :],
                                                            op=ALU.mult),
                          reads=['accA', (rd.name, 0)], writes=[(mt.name, 0)])
                    kb.op('pool', lambda e: e.tensor_tensor(out=mt[64:128, :], in0=acc[1][64:128, cs],
                                                            in1=rd[64:128, :], op=ALU.mult),
                          reads=['accB', (rd.name, 1)], writes=[(mt.name, 1)])
                    kb.dma('sp', scr['mixT'][512 + hp * 128:512 + (hp + 1) * 128, bb + c * 512:bb + (c + 1) * 512],
                           mt[:], reads=[(mt.name, 0), (mt.name, 1)], writes=[kb.uq()])
        kb.barrier()


def make_cmask():
    c = np.arange(128)[:, None]
    i = np.arange(128)[None, :]
    NEG = -1.0e5
    triF = (c <= i).astype(np.float32)
    triB = (c >= i).astype(np.float32)
    mLf = np.where(i < c, 0.0, NEG)
    mLb = np.where(i > c, 0.0, NEG)
    mATf = np.where(c <= i, 0.0, NEG)
    mATb = np.where(c >= i, 0.0, NEG)
    return np.ascontiguousarray(np.stack([triF, triB, mLf, mLb, mATf, mATb], axis=1).astype(np.float32))


def phase1b(kb, cfg, io, scr):
    nc = kb.nc
    WU = 256
    maxR = max((b + 2 * WU) if f else b for b, f in cfg.segs)
    nchm = maxR // 128
    with contextlib.ExitStack() as es:
        def sb(name, shape, dt):
            return es.enter_context(nc.sbuf_tensor("b_" + name, list(shape), dt))

        banks = [es.enter_context(nc.psum_tensor("bk%d" % i, [128, 512], F32)) for i in range(8)]
        cm = sb("cm", [128, 6, 128], F32)
        identb = sb("identb", [128, 128], BF16)
        ones_f = sb("ones_f", [128, 128], F32)
        dnc = sb("dnc", [128, 16], F32)
        negA = sb("negA", [128, 8], F32)
        convw = sb("convw", [128, 12, 3], F32)
        gn = sb("gn", [128, 128], F32)
        kb.dma('sp', cm[:], io['cmask'][:, :, :], writes=['cm'])
        kb.dma('sp', identb[:], io['ident_bf'][:, :], writes=['identb'])
        kb.dma('sp', dnc[:], io['dnc'][:, :], writes=['dnc'])
        kb.dma('sp', convw[:], io['convw'][:, :, :], writes=['convw'])
        kb.dma('sp', gn[:], io['gn'][:, :], writes=['gn'])
        kb.op('pool', lambda e: e.memset(ones_f[:], 1.0), writes=['ones_f'])
        kb.op('act', lambda e: e.activation(out=negA[:], in_=dnc[:, 8:16], func=AF.Exp), reads=['dnc'], writes=['negA'])
        kb.op('dve', lambda e: e.tensor_scalar(out=negA[:], in0=negA[:], scalar1=-1.0, scalar2=None, op0=ALU.mult),
              reads=['negA'], writes=['negA'])

        def sct(name):
            return sb(name, [128, nchm, 8], F32)

        sc_t = sb("sc_t", [128, nchm, 16], F32)
        beta, gg, Gtok, Gtot, expG, expGtot, kdec, negG, coef, spx, spl, GLb = [
            sct(n) for n in ("beta", "gg", "Gtok", "Gtot", "expG", "expGtot", "kdec", "negG", "coef", "spx", "spl", "GLb")]
        CB = min(3072, maxR)
        xin = sb("xin", [128, CB + 2], F32)
        cv = sb("cv", [128, CB], F32)
        qT = sb("dqT", [128, maxR], BF16)
        kT = sb("dkT", [128, maxR], BF16)
        Ktok = sb("Ktok", [128, nchm, 128], BF16)
        Vtok = sb("Vtok", [128, nchm, 128], BF16)
        Osum = sb("Osum", [128, nchm, 128], F32)
        vTfull = Osum[:].rearrange("p c k -> p (c k)").bitcast(BF16)
        sqs = Rot([sb("sq%d" % i, [128, 512], F32) for i in range(2)])
        rss = Rot([sb("rs%d" % i, [128, 512], F32) for i in range(2)])
        S32 = [sb("S32_%d" % i, [128, 128], F32) for i in range(2)]
        Sbf = [sb("Sbf_%d" % i, [128, 128], BF16) for i in range(2)]

        def ring(name, dt, n=2, shape=(128, 128)):
            return [Rot([sb("%s_%d_%d" % (name, dr, i), list(shape), dt) for i in range(n)]) for dr in range(2)]

        G4 = (128, 4, 128)
        r_gtri = ring("gtri", F32, n=1, shape=G4)
        r_t1 = ring("t1", F32, n=1, shape=G4)
        r_t2 = ring("t2", F32, n=1, shape=G4)
        r_L = ring("Lm", BF16, n=2, shape=G4)
        r_AT = ring("ATm", BF16, n=3, shape=G4)
        r_P = ring("Pm", BF16, n=3, shape=G4)
        r_Q = ring("Qm", BF16, n=3, shape=G4)
        r_X = ring("Xm", BF16, n=3, shape=G4)
        r_TT = ring("TTm", BF16, n=3, shape=G4)
        r_Kd = ring("Kd", BF16, n=3, shape=G4)
        r_Vb = ring("Vb", BF16, n=3, shape=G4)
        r_R = ring("Rm", BF16)
        r_Vn = ring("Vn", BF16)
        r_QSs = ring("QSs", F32)
        r_tO = ring("tO", F32)
        zts = Rot([sb("zt%d" % i, [128, 128], F32) for i in range(3)])
        szs = Rot([sb("sz%d" % i, [128, 128], F32) for i in range(3)])
        gts = Rot([sb("gt%d" % i, [128, 128], F32) for i in range(3)])
        g2s = Rot([sb("g2%d" % i, [128, 128], BF16) for i in range(3)])
        gss = Rot([sb("gs%d" % i, [128, 4], F32) for i in range(3)])
        gjunk = sb("gjunk", [128, 128], F32)
        ostg = Rot([sb("ostg%d" % i, [128, 512], BF16) for i in range(2)])

        for s, (body, full) in enumerate(cfg.segs):
            R = (body + 2 * WU) if full else body
            R0 = cfg.slot_base[s] + ((H - WU) if full else H)
            nch = R // 128
            ch_b0 = (WU // 128) if full else 0
            nch_b = body // 128
            bb = body_base(cfg, s)
            NC8 = nch * 8

            def fl(t):
                return t[:, 0:nch, :].rearrange("p c k -> p (c k)")

            kb.dma('sp', sc_t[:, 0:nch, :], scr['sc'][R0:R0 + R, :].rearrange("(c p) k -> p c k", p=128),
                   reads=[('sc', s)], writes=['sc_t'])
            kb.op('act', lambda e: e.activation(out=beta[:, 0:nch, :], in_=sc_t[:, 0:nch, 0:8], func=AF.Sigmoid),
                  reads=['sc_t'], writes=['beta'])
            kb.op('dve', lambda e: e.tensor_tensor(out=spx[:, 0:nch, :], in0=sc_t[:, 0:nch, 8:16],
                                                   in1=dnc[:, 0:8].unsqueeze(1).to_broadcast([128, nch, 8]), op=ALU.add),
                  reads=['sc_t', 'dnc'], writes=['spx'])
            kb.op('dve', lambda e: e.scalar_tensor_tensor(out=spl[:, 0:nch, :], in0=spx[:, 0:nch, :], scalar=-1.0,
                                                          in1=spx[:, 0:nch, :], op0=ALU.mult, op1=ALU.max),
                  reads=['spx'], writes=['spl'])
            kb.op('act', lambda e: e.activation(out=spl[:, 0:nch, :], in_=spl[:, 0:nch, :], func=AF.Exp, scale=-1.0),
                  reads=['spl'], writes=['spl'])
            kb.op('act', lambda e: e.activation(out=spl[:, 0:nch, :], in_=spl[:, 0:nch, :], func=AF.Ln, bias=1.0),
                  reads=['spl'], writes=['spl'])
            kb.op('dve', lambda e: e.scalar_tensor_tensor(out=spx[:, 0:nch, :], in0=spx[:, 0:nch, :], scalar=0.0,
                                                          in1=spl[:, 0:nch, :], op0=ALU.max, op1=ALU.add),
                  reads=['spx', 'spl'], writes=['spx'])
            kb.op('dve', lambda e: e.tensor_tensor(out=gg[:, 0:nch, :], in0=spx[:, 0:nch, :],
                                                   in1=negA[:, 0:8].unsqueeze(1).to_broadcast([128, nch, 8]), op=ALU.mult),
                  reads=['spx', 'negA'], writes=['gg'])
            for ch in range(nch):
                kb.op('pe', lambda e: e.matmul(banks[0][:, ch * 8:ch * 8 + 4], lhsT=cm[:, 0, :], rhs=gg[:, ch, 0:4],
                                               start=True, stop=True), reads=['gg', 'cm'], writes=['bk0'])
                kb.op('pe', lambda e: e.matmul(banks[0][:, ch * 8 + 4:ch * 8 + 8], lhsT=cm[:, 1, :], rhs=gg[:, ch, 4:8],
                                               start=True, stop=True), reads=['gg', 'cm'], writes=['bk0'])
                kb.op('pe', lambda e: e.matmul(banks[1][:, ch * 8:ch * 8 + 8], lhsT=ones_f[:], rhs=gg[:, ch, :],
                                               start=True, stop=True), reads=['gg', 'ones_f'], writes=['bk1'])
            kb.op('dve', lambda e: e.tensor_copy(out=fl(Gtok), in_=banks[0][:, 0:NC8]), reads=['bk0'], writes=['Gtok'])
            kb.op('dve', lambda e: e.tensor_copy(out=fl(Gtot), in_=banks[1][:, 0:NC8]), reads=['bk1'], writes=['Gtot'])
            kb.op('act', lambda e: e.activation(out=fl(expG), in_=fl(Gtok), func=AF.Exp), reads=['Gtok'], writes=['expG'])
            kb.op('act', lambda e: e.activation(out=fl(expGtot), in_=fl(Gtot), func=AF.Exp), reads=['Gtot'],
                  writes=['expGtot'])
            kb.op('dve', lambda e: e.tensor_tensor(out=fl(kdec), in0=fl(Gtot), in1=fl(Gtok), op=ALU.subtract),
                  reads=['Gtot', 'Gtok'], writes=['kdec'])
            kb.op('act', lambda e: e.activation(out=fl(kdec), in_=fl(kdec), func=AF.Exp), reads=['kdec'], writes=['kdec'])
            kb.op('dve', lambda e: e.tensor_scalar(out=fl(negG), in0=fl(Gtok), scalar1=-1.0, scalar2=None, op0=ALU.mult),
                  reads=['Gtok'], writes=['negG'])
            kb.op('dve', lambda e: e.scalar_tensor_tensor(out=fl(coef), in0=fl(beta), scalar=-1.0, in1=fl(expG),
                                                          op0=ALU.mult, op1=ALU.mult),
                  reads=['beta', 'expG'], writes=['coef'])
            kb.op('act', lambda e: e.activation(out=fl(GLb), in_=fl(beta), func=AF.Ln), reads=['beta'], writes=['GLb'])
            kb.op('dve', lambda e: e.tensor_tensor(out=fl(GLb), in0=fl(GLb), in1=fl(Gtok), op=ALU.add),
                  reads=['GLb', 'Gtok'], writes=['GLb'])
            kb.barrier()

            for hd in range(4):
                for idx in range(3):
                    ct = idx * 4 + hd
                    row0 = idx * 512 + hd * 128
                    for c0 in range(0, R, CB):
                        cb = min(CB, R - c0)
                        lo_ = 1 if (c0 == 0 and not full) else 0
                        hi_ = 1 if (c0 + cb == R and not full) else 0
                        if lo_:
                            kb.op('pool', lambda e: e.memset(xin[:, 0:1], 0.0), writes=['xin'])
                        if hi_:
                            kb.op('pool', lambda e: e.memset(xin[:, cb + 1:cb + 2], 0.0), writes=['xin'])
                        kb.dma('sp', xin[:, lo_:cb + 2 - hi_],
                               scr['dnT'][row0:row0 + 128, R0 + c0 - 1 + lo_:R0 + c0 + cb + 1 - hi_],
                               reads=[('dnT', s)], writes=['xin'])
                        kb.op('dve', lambda e: e.tensor_scalar(out=cv[:, 0:cb], in0=xin[:, 0:cb], scalar1=convw[:, ct, 0:1],
                                                               scalar2=None, op0=ALU.mult),
                              reads=['xin', 'convw'], writes=['cv'])
                        kb.op('dve', lambda e: e.scalar_tensor_tensor(out=cv[:, 0:cb], in0=xin[:, 1:cb + 1],
                                                                      scalar=convw[:, ct, 1:2], in1=cv[:, 0:cb],
                                                                      op0=ALU.mult, op1=ALU.add),
                              reads=['xin', 'convw', 'cv'], writes=['cv'])
                        kb.op('dve', lambda e: e.scalar_tensor_tensor(out=cv[:, 0:cb], in0=xin[:, 2:cb + 2],
                                                                      scalar=convw[:, ct, 2:3], in1=cv[:, 0:cb],
                                                                      op0=ALU.mult, op1=ALU.add),
                              reads=['xin', 'convw', 'cv'], writes=['cv'])
                        if idx == 2:
                            kb.op('act', lambda e: e.activation(out=vTfull[:, c0:c0 + cb], in_=cv[:, 0:cb], func=AF.Silu),
                                  reads=['cv'], writes=['vT'])
                            continue
                        dst = qT if idx == 0 else kT
                        kb.op('act', lambda e: e.activation(out=cv[:, 0:cb], in_=cv[:, 0:cb], func=AF.Silu),
                              reads=['cv'], writes=['cv'])
                        for blk in range(cb // 512):
                            bs = slice(blk * 512, (blk + 1) * 512)
                            ds_ = slice(c0 + blk * 512, c0 + (blk + 1) * 512)
                            sq = sqs.next()
                            rs = rss.next()
                            bk = banks[2 + blk % 2]
                            kb.op('pool', lambda e: e.tensor_tensor(out=sq[:], in0=cv[:, bs], in1=cv[:, bs], op=ALU.mult),
                                  reads=['cv'], writes=[sq.name])
                            kb.op('pe', lambda e: e.matmul(bk[:, :], lhsT=ones_f[:], rhs=sq[:], start=True, stop=True),
                                  reads=[sq.name, 'ones_f'], writes=[bk.name])
                            kb.op('act', lambda e: e.activation(out=rs[:], in_=bk[:, :], func=AF.Sqrt, bias=EPS),
                                  reads=[bk.name], writes=[rs.name])
                            kb.op('dve', lambda e: e.reciprocal(out=rs[:], in_=rs[:]), reads=[rs.name], writes=[rs.name])
                            sc_ = (128.0 ** -0.5) if idx == 0 else 1.0
                            kb.op('dve', lambda e: e.scalar_tensor_tensor(out=dst[:, ds_], in0=cv[:, bs], scalar=sc_,
                                                                          in1=rs[:], op0=ALU.mult, op1=ALU.mult),
                                  reads=['cv', rs.name], writes=[dst.name])
                kb.barrier()
                for src, srck, dstt, bi in ((kT[:, :], kT.name, Ktok, 0), (vTfull, 'vT', Vtok, 1)):
                    for g0 in range(0, nch, 8):
                        gn_ = min(8, nch - g0)
                        bk = banks[bi * 2 + (g0 // 8) % 2]
                        bkb = bk[:].bitcast(BF16)
                        for c in range(gn_):
                            ch = g0 + c
                            kb.op('pe', lambda e: e.transpose(out=bkb[:, c * 128:(c + 1) * 128],
                                                              in_=src[:, ch * 128:(ch + 1) * 128], identity=identb[:]),
                                  reads=[srck, 'identb'], writes=[bk.name])
                        kb.op('dve', lambda e: e.tensor_copy(
                            out=dstt[:, g0:g0 + gn_, :].rearrange("p c k -> p (c k)"), in_=bkb[:, 0:gn_ * 128]),
                            reads=[bk.name], writes=[dstt.name])
                kb.barrier()
                kb.op('pool', lambda e: e.memset(Osum[:, 0:nch, :], 0.0), writes=['Osum'])
                for dr in range(2):
                    kb.op('pool', lambda e: e.memset(S32[dr][:], 0.0), writes=['S32_%d' % dr])
                    kb.op('pool', lambda e: e.memset(Sbf[dr][:], 0.0), writes=['Sbf_%d' % dr])
                kb.barrier()

                GB = 4
                pre = {}

                def bc_s(t, c0, col):
                    return t[:, c0:c0 + GB, col:col + 1].to_broadcast([128, GB, 128])

                def bc_m(k):
                    return cm[:, k:k + 1, :].to_broadcast([128, GB, 128])

                def v4(bank):
                    return bank[:, :].rearrange("p (g k) -> p g k", g=GB)

                def precompute(c0, dr):
                    col = dr * 4 + hd
                    MD = banks[4 * dr + 0]
                    XD = MD
                    PA = banks[4 * dr + 1]
                    QA = banks[4 * dr + 2]
                    gtri = r_gtri[dr].next(); t1 = r_t1[dr].next(); t2 = r_t2[dr].next()
                    Lm = r_L[dr].next(); ATm = r_AT[dr].next()
                    kb.op('dve', lambda e: e.tensor_tensor(out=gtri[:], in0=bc_m(dr), in1=bc_s(gg, c0, col), op=ALU.mult),
                          reads=['cm', 'gg'], writes=[gtri.name])
                    for i in range(GB):
                        kb.op('pe', lambda e: e.matmul(MD[:, i * 128:(i + 1) * 128], lhsT=ones_f[:], rhs=gtri[:, i, :],
                                                       start=True, stop=True),
                              reads=['ones_f', gtri.name], writes=[MD.name])
                    kb.op('dve', lambda e: e.tensor_tensor(out=t1[:], in0=bc_m(2 + dr), in1=v4(MD), op=ALU.subtract),
                          reads=['cm', MD.name], writes=[t1.name])
                    kb.op('dve', lambda e: e.tensor_tensor(out=t2[:], in0=bc_m(4 + dr), in1=v4(MD), op=ALU.add),
                          reads=['cm', MD.name], writes=[t2.name])
                    kb.op('dve', lambda e: e.tensor_tensor(out=t1[:], in0=t1[:], in1=bc_s(GLb, c0, col), op=ALU.add),
                          reads=[t1.name, 'GLb'], writes=[t1.name])
                    kb.op('dve', lambda e: e.tensor_tensor(out=t2[:], in0=t2[:], in1=bc_s(negG, c0, col), op=ALU.add),
                          reads=[t2.name, 'negG'], writes=[t2.name])
                    kb.op('act', lambda e: e.activation(out=t1[:], in_=t1[:], func=AF.Exp), reads=[t1.name], writes=[t1.name])
                    kb.op('act', lambda e: e.activation(out=t2[:], in_=t2[:], func=AF.Exp), reads=[t2.name], writes=[t2.name])
                    yield
                    for i in range(GB):
                        cs = slice((c0 + i) * 128, (c0 + i + 1) * 128)
                        kb.op('pe', lambda e: e.matmul(MD[:, i * 128:(i + 1) * 128], lhsT=kT[:, cs], rhs=kT[:, cs],
                                                       start=True, stop=True), reads=[kT.name], writes=[MD.name])
                    kb.op('dve', lambda e: e.tensor_tensor(out=Lm[:], in0=v4(MD), in1=t1[:], op=ALU.mult),
                          reads=[MD.name, t1.name], writes=[Lm.name])
                    for i in range(GB):
                        cs = slice((c0 + i) * 128, (c0 + i + 1) * 128)
                        kb.op('pe', lambda e: e.matmul(XD[:, i * 128:(i + 1) * 128], lhsT=kT[:, cs], rhs=qT[:, cs],
                                                       start=True, stop=True), reads=[kT.name, qT.name], writes=[XD.name])
                    kb.op('dve', lambda e: e.tensor_tensor(out=ATm[:], in0=v4(XD), in1=t2[:], op=ALU.mult),
                          reads=[XD.name, t2.name], writes=[ATm.name])
                    MDb = MD[:, :].bitcast(BF16)
                    for i in range(GB):
                        kb.op('pe', lambda e: e.transpose(out=MDb[:, i * 128:(i + 1) * 128], in_=Lm[:, i, :],
                                                          identity=identb[:]),
                              reads=[Lm.name, 'identb'], writes=[MD.name])
                    NTv = MDb[:, 0:GB * 128].rearrange("p (g k) -> p g k", g=GB)
                    P = r_P[dr].next(); X = r_X[dr].next(); Q = Lm
                    kb.op('dve', lambda e: e.tensor_copy(out=P[:], in_=NTv), reads=[MD.name], writes=[P.name])
                    kb.op('dve', lambda e: e.tensor_tensor(out=X[:], in0=identb[:].unsqueeze(1).to_broadcast([128, GB, 128]),
                                                           in1=NTv, op=ALU.subtract),
                          reads=[MD.name, 'identb'], writes=[X.name])
                    yield
                    for lvl in range(1, 7):
                        Pn = r_P[dr].next() if lvl < 6 else None
                        Qn = r_Q[dr].next()
                        if Pn is not None:
                            for i in range(GB):
                                kb.op('pe', lambda e: e.matmul(PA[:, i * 128:(i + 1) * 128], lhsT=Q[:, i, :], rhs=P[:, i, :],
                                                               start=True, stop=True),
                                      reads=[Q.name, P.name], writes=[PA.name])
                        for i in range(GB):
                            kb.op('pe', lambda e: e.matmul(QA[:, i * 128:(i + 1) * 128], lhsT=P[:, i, :], rhs=Q[:, i, :],
                                                           start=True, stop=True),
                                  reads=[Q.name, P.name], writes=[QA.name])
                        yield
                        if Pn is not None:
                            kb.op('act', lambda e: e.copy(out=Pn[:], in_=v4(PA)), reads=[PA.name], writes=[Pn.name])
                        kb.op('act', lambda e: e.copy(out=Qn[:], in_=v4(QA)), reads=[QA.name], writes=[Qn.name])
                        Xn = r_X[dr].next() if lvl < 6 else r_TT[dr].next()
                        for i in range(GB):
                            kb.op('pe', lambda e: e.matmul(XD[:, i * 128:(i + 1) * 128], lhsT=Qn[:, i, :], rhs=X[:, i, :],
                                                           start=True, stop=True),
                                  reads=[Qn.name, X.name], writes=[XD.name])
                        kb.op('dve', lambda e: e.tensor_tensor(out=Xn[:], in0=X[:], in1=v4(XD), op=ALU.add),
                              reads=[X.name, XD.name], writes=[Xn.name])
                        P, Q, X = Pn, Qn, Xn
                        yield
                    Kd = r_Kd[dr].next(); Vb = r_Vb[dr].next()
                    kb.op('pool', lambda e: e.tensor_tensor(out=Kd[:], in0=Ktok[:, c0:c0 + GB, :], in1=bc_s(kdec, c0, col),
                                                            op=ALU.mult),
                          reads=[Ktok.name, 'kdec'], writes=[Kd.name])
                    kb.op('pool', lambda e: e.tensor_tensor(out=Vb[:], in0=Vtok[:, c0:c0 + GB, :], in1=bc_s(beta, c0, col),
                                                            op=ALU.mult),
                          reads=[Vtok.name, 'beta'], writes=[Vb.name])
                    pre[(c0, dr)] = (X, ATm, Kd, Vb)
                    yield

                def chain(ch, dr):
                    col = dr * 4 + hd
                    cs = slice(ch * 128, (ch + 1) * 128)
                    B3 = banks[4 * dr + 3]
                    n3 = B3.name
                    c0 = (ch // GB) * GB
                    i = ch - c0
                    TT, ATm, Kd, Vb = pre[(c0, dr)]
                    Rm = r_R[dr].next(); Vn = r_Vn[dr].next(); QSs = r_QSs[dr].next(); tO = r_tO[dr].next()
                    sn, sbn = 'S32_%d' % dr, 'Sbf_%d' % dr
                    kb.op('pe', lambda e: e.matmul(B3[:, 0:128], lhsT=kT[:, cs], rhs=Sbf[dr][:], start=True, stop=True),
                          reads=[kT.name, sbn], writes=[n3])
                    kb.op('pe', lambda e: e.matmul(B3[:, 128:256], lhsT=qT[:, cs], rhs=Sbf[dr][:], start=True, stop=True),
                          reads=[qT.name, sbn], writes=[n3])
                    yield
                    kb.op('dve', lambda e: e.scalar_tensor_tensor(out=Rm[:], in0=B3[:, 0:128],
                                                                  scalar=coef[:, ch, col:col + 1], in1=Vb[:, i, :],
                                                                  op0=ALU.mult, op1=ALU.add),
                          reads=[n3, 'coef', Vb.name], writes=[Rm.name])
                    kb.op('dve', lambda e: e.tensor_scalar(out=QSs[:], in0=B3[:, 128:256],
                                                           scalar1=expG[:, ch, col:col + 1], scalar2=None, op0=ALU.mult),
                          reads=[n3, 'expG'], writes=[QSs.name])
                    kb.op('pe', lambda e: e.matmul(B3[:, 256:384], lhsT=TT[:, i, :], rhs=Rm[:], start=True, stop=True),
                          reads=[TT.name, Rm.name], writes=[n3])
                    yield
                    kb.op('dve', lambda e: e.tensor_copy(out=Vn[:], in_=B3[:, 256:384]), reads=[n3], writes=[Vn.name])
                    kb.op('pe', lambda e: e.matmul(B3[:, 0:128], lhsT=ATm[:, i, :], rhs=Vn[:], start=True, stop=True),
                          reads=[ATm.name, Vn.name], writes=[n3])
                    kb.op('pe', lambda e: e.matmul(B3[:, 384:512], lhsT=Kd[:, i, :], rhs=Vn[:], start=True, stop=True),
                          reads=[Kd.name, Vn.name], writes=[n3])
                    yield
                    kb.op('dve', lambda e: e.tensor_tensor(out=tO[:], in0=QSs[:], in1=B3[:, 0:128], op=ALU.add),
                          reads=[QSs.name, n3], writes=[tO.name])
                    kb.op('pool', lambda e: e.tensor_tensor(out=Osum[:, ch, :], in0=Osum[:, ch, :], in1=tO[:], op=ALU.add),
                          reads=[tO.name, ('Osum', ch)], writes=[('Osum', ch)])
                    kb.op('dve', lambda e: e.scalar_tensor_tensor(out=S32[dr][:], in0=S32[dr][:],
                                                                  scalar=expGtot[:, ch, col:col + 1], in1=B3[:, 384:512],
                                                                  op0=ALU.mult, op1=ALU.add),
                          reads=[sn, 'expGtot', n3], writes=[sn])
                    kb.op('act', lambda e: e.copy(out=Sbf[dr][:], in_=S32[dr][:]), reads=[sn], writes=[sbn])
                    yield

                nblk = nch // GB
                fblocks = [b_ * GB for b_ in range(nblk)]
                bblocks = [(nblk - 1 - b_) * GB for b_ in range(nblk)]

                def chainseq(c0, dr):
                    order_ = range(GB) if dr == 0 else range(GB - 1, -1, -1)
                    for i_ in order_:
                        for _ in chain(c0 + i_, dr):
                            yield

                gens = [precompute(fblocks[0], 0), precompute(bblocks[0], 1)]
                while gens:
                    for g_ in list(gens):
                        try:
                            next(g_)
                        except StopIteration:
                            gens.remove(g_)
                for t in range(nblk if not getattr(cfg, 'dn_nochain', False) else 0):
                    gens = [chainseq(fblocks[t], 0), chainseq(bblocks[t], 1)]
                    if t + 1 < nblk:
                        gens += [precompute(fblocks[t + 1], 0), precompute(bblocks[t + 1], 1)]
                    while gens:
                        for g_ in list(gens):
                            try:
                                next(g_)
                            except StopIteration:
                                gens.remove(g_)
                    pre.pop((fblocks[t], 0), None)
                    pre.pop((bblocks[t], 1), None)
                kb.barrier()

                for g0 in range(0, nch_b, 4):
                    bk = banks[(g0 // 4) % 2]
                    bkb = bk[:].bitcast(BF16)
                    gc_ = min(4, nch_b - g0)
                    for c in range(gc_):
                        ch = ch_b0 + g0 + c
                        zt = zts.next(); sz = szs.next(); gt = gts.next(); g2 = g2s.next(); gs = gss.next()
                        t0 = R0 + ch * 128
                        kb.dma('sp', zt[:], scr['zs'][t0:t0 + 128, hd * 128:(hd + 1) * 128],
                               reads=[('zs', s)], writes=[zt.name])
                        kb.op('act', lambda e: e.activation(out=sz[:], in_=zt[:], func=AF.Silu),
                              reads=[zt.name], writes=[sz.name])
                        kb.op('act', lambda e: e.activation(out=gjunk[:], in_=Osum[:, ch, :], func=AF.Square,
                                                            accum_out=gs[:, 0:1]),
                              reads=[('Osum', ch)], writes=['gjunk', gs.name])
                        kb.op('act', lambda e: e.activation(out=gs[:, 1:2], in_=gs[:, 0:1], func=AF.Sqrt,
                                                            scale=1.0 / 128, bias=EPS),
                              reads=[gs.name], writes=[gs.name])
                        kb.op('dve', lambda e: e.reciprocal(out=gs[:, 2:3], in_=gs[:, 1:2]), reads=[gs.name],
                              writes=[gs.name])
                        kb.op('dve', lambda e: e.scalar_tensor_tensor(out=gt[:], in0=Osum[:, ch, :], scalar=gs[:, 2:3],
                                                                      in1=gn[:], op0=ALU.mult, op1=ALU.mult),
                              reads=[('Osum', ch), gs.name, 'gn'], writes=[gt.name])
                        kb.op('pool', lambda e: e.tensor_tensor(out=g2[:], in0=gt[:], in1=sz[:], op=ALU.mult),
                              reads=[gt.name, sz.name], writes=[g2.name])
                        kb.op('pe', lambda e: e.transpose(out=bkb[:, c * 128:(c + 1) * 128], in_=g2[:], identity=identb[:]),
                              reads=[g2.name, 'identb'], writes=[bk.name])
                    og = ostg.next()
                    kb.op('dve', lambda e: e.tensor_copy(out=og[:, 0:gc_ * 128], in_=bkb[:, 0:gc_ * 128]), reads=[bk.name], writes=[og.name])
                    c0 = bb + g0 * 128
                    kb.dma('sp', scr['mixT'][hd * 128:(hd + 1) * 128, c0:c0 + gc_ * 128], og[:, 0:gc_ * 128],
                           reads=[og.name], writes=[kb.uq()])
                kb.barrier()
        kb.barrier()


def phase1d(kb, cfg, io, scr, outs):
    nc = kb.nc
    with contextlib.ExitStack() as es:
        def sb(name, shape, dt):
            return es.enter_context(nc.sbuf_tensor("d_" + name, list(shape), dt))

        banks = [es.enter_context(nc.psum_tensor("dbk%d" % i, [128, 512], F32)) for i in range(8)]
        wo = sb("wo", [128, 8, D], BF16)
        wst = [sb("wst%d" % i, [128, D], F32) for i in range(2)]
        wr = sb("wr", [128, 8, 16], F32)
        g2b = sb("g2b", [128, D], F32)
        identf = sb("identf", [128, 128], F32)
        mxs = Rot([sb("mx%d" % i, [128, 8, 512], BF16) for i in range(2)])
        xts = Rot([sb("xt%d" % i, [128, D], F32) for i in range(3)])
        x1s = Rot([sb("x1_%d" % i, [128, D], F32) for i in range(4)])
        h2fs = Rot([sb("h2f%d" % i, [128, D], F32) for i in range(5)])
        h2bs = Rot([sb("h2b%d" % i, [128, D], BF16) for i in range(2)])
        h2Ts = Rot([sb("h2T%d" % i, [128, 8, 128], F32) for i in range(2)])
        junk = sb("junk", [128, D], BF16)
        sss = Rot([sb("ss%d" % i, [128, 8], F32) for i in range(6)])
        lgs = Rot([sb("lg%d" % i, [128, 16], F32) for i in range(3)])
        afs = Rot([sb("af%d" % i, [128, 16], F32) for i in range(3)])

        kb.dma('sp', g2b[:], io['g2b'][:, :], writes=['g2b'])
        kb.dma('sp', identf[:], io['ident_f'][:, :], writes=['identf'])
        kb.dma('sp', wr[:], io['w_router'].rearrange("(k p) e -> p k e", p=128), writes=['wr'])
        for k in range(8):
            w = wst[k % 2]
            kb.dma('sp', w[:], io['w_out'][k * 128:(k + 1) * 128, :], writes=[w.name])
            kb.op('dve', lambda e: e.tensor_copy(out=wo[:, k, :], in_=w[:]), reads=[w.name], writes=[('wo', k)])
        wokeys = [('wo', k) for k in range(8)]
        pi = [0]
        for s, (body, full) in enumerate(cfg.segs):
            bb = body_base(cfg, s)
            a0 = cfg.act_base[s] + (H if full else 0)
            for g in range(body // 512):
                mx = mxs.next()
                kb.dma('sp', mx[:], scr['mixT'][:, bb + g * 512:bb + (g + 1) * 512].rearrange("(k p) t -> p k t", p=128),
                       reads=[('mixT', s)], writes=[mx.name])
                def tile_(j):
                    r0 = a0 + g * 512 + j * 128
                    o0 = bb + g * 512 + j * 128
                    xt = xts.next(); x1 = x1s.next(); h2f = h2fs.next(); h2b = h2bs.next(); h2T = h2Ts.next()
                    ss = sss.next(); lg = lgs.next(); af = afs.next()
                    kb.dma('sp', xt[:], io['xs'][r0:r0 + 128, :], writes=[xt.name])
                    for hf in range(2):
                        bk = banks[pi[0] % 4]
                        pi[0] += 1
                        for k in range(8):
                            kb.op('pe', lambda e: e.matmul(bk[:, :], lhsT=mx[:, k, j * 128:(j + 1) * 128],
                                                           rhs=wo[:, k, hf * 512:(hf + 1) * 512],
                                                           start=(k == 0), stop=(k == 7)),
                                  reads=[mx.name, wokeys[k]], writes=[bk.name])
                        kb.op('dve', lambda e: e.tensor_tensor(out=x1[:, hf * 512:(hf + 1) * 512],
                                                               in0=xt[:, hf * 512:(hf + 1) * 512], in1=bk[:, :],
                                                               op=ALU.add),
                              reads=[xt.name, bk.name], writes=[(x1.name, hf)])
                    x1k = [(x1.name, 0), (x1.name, 1)]
                    kb.dma('sp', outs['x1'][o0:o0 + 128, :], x1[:], reads=x1k, writes=['out_x1'])
                    kb.op('act', lambda e: e.activation(out=junk[:], in_=x1[:], func=AF.Square, accum_out=ss[:, 0:1]),
                          reads=x1k, writes=['junk', ss.name])
                    kb.op('act', lambda e: e.activation(out=ss[:, 1:2], in_=ss[:, 0:1], func=AF.Sqrt, scale=1.0 / D,
                                                        bias=EPS), reads=[ss.name], writes=[ss.name])
                    kb.op('dve', lambda e: e.reciprocal(out=ss[:, 2:3], in_=ss[:, 1:2]), reads=[ss.name],
                          writes=[ss.name])
                    kb.op('dve', lambda e: e.scalar_tensor_tensor(out=h2f[:], in0=x1[:], scalar=ss[:, 2:3], in1=g2b[:],
                                                                  op0=ALU.mult, op1=ALU.mult),
                          reads=x1k + [ss.name, 'g2b'], writes=[h2f.name])
                    kb.op('act', lambda e: e.copy(out=h2b[:], in_=h2f[:]), reads=[h2f.name], writes=[h2b.name])
                    kb.dma('sp', outs['h2'][o0:o0 + 128, :], h2b[:], reads=[h2b.name], writes=['out_h2'])
                    yield
                    for half in range(2):
                        bk = banks[4 + half]
                        for kk in range(4):
                            k = half * 4 + kk
                            kb.op('pe', lambda e: e.transpose(out=bk[:, kk * 128:(kk + 1) * 128],
                                                              in_=h2f[:, k * 128:(k + 1) * 128], identity=identf[:]),
                                  reads=[h2f.name, 'identf'], writes=[bk.name])
                        kb.op('dve', lambda e: e.tensor_copy(
                            out=h2T[:, half * 4:(half + 1) * 4, :].rearrange("p k t -> p (k t)"), in_=bk[:, :]),
                            reads=[bk.name], writes=[(h2T.name, half)])
                    bk = banks[6 + (pi[0] % 2)]
                    for k in range(8):
                        kb.op('pe', lambda e: e.matmul(bk[:, 0:16], lhsT=h2T[:, k, :], rhs=wr[:, k, :],
                                                       start=(k == 0), stop=(k == 7)),
                              reads=[(h2T.name, 0), (h2T.name, 1), 'wr'], writes=[bk.name])
                    kb.op('dve', lambda e: e.tensor_copy(out=lg[:], in_=bk[:, 0:16]), reads=[bk.name], writes=[lg.name])
                    kb.op('dve', lambda e: e.reduce_max(out=ss[:, 3:4], in_=lg[:], axis=AX.X), reads=[lg.name],
                          writes=[ss.name])
                    kb.op('dve', lambda e: e.tensor_scalar(out=ss[:, 4:5], in0=ss[:, 3:4], scalar1=-1.0, scalar2=None,
                                                           op0=ALU.mult), reads=[ss.name], writes=[ss.name])
                    kb.op('act', lambda e: e.activation(out=af[:], in_=lg[:], func=AF.Exp, bias=ss[:, 4:5],
                                                        accum_out=ss[:, 5:6]),
                          reads=[lg.name, ss.name], writes=[af.name, ss.name])
                    kb.op('dve', lambda e: e.reciprocal(out=ss[:, 6:7], in_=ss[:, 5:6]), reads=[ss.name],
                          writes=[ss.name])
                    kb.op('dve', lambda e: e.tensor_scalar(out=af[:], in0=af[:], scalar1=ss[:, 6:7], scalar2=None,
                                                           op0=ALU.mult), reads=[af.name, ss.name], writes=[af.name])
                    kb.dma('sp', outs['aff'][o0:o0 + 128, :], af[:], reads=[af.name], writes=['out_aff'])
                    yield
                gens_ = [tile_(j) for j in range(4)]
                for g_ in gens_:
                    next(g_)
                for g_ in gens_:
                    next(g_)
        kb.barrier()


NEXP = 16
FF = 2816
NFT = FF // 128


class Cfg2:
    def __init__(self, nb, group_tiles, nall, topk, cap, ff=2816):
        self.FF = ff
        self.NB = nb
        self.ntt = nb // 128
        self.group_tiles = group_tiles
        self.NA = nall // 128
        self.topk = topk
        self.CAP = cap
        self.CAPP = cap + 128


FULL_CFG2 = Cfg2(8192, [(0, 32), (32, 64)], 32768, 4096, 1152)


def build_program2(c2, debug=False):
    nc = bass.Bass("TRN2", target_bir_lowering=False)
    kb = KB(nc)
    io = {}

    def inp(name, shape, dt=F32):
        io[name] = nc.dram_tensor(name, list(shape), dt, kind="ExternalInput").ap()

    NB, ntt, NA, CAP, CAPP = c2.NB, c2.ntt, c2.NA, c2.CAP, c2.CAPP
    FF = c2.FF
    NFT = FF // 128
    nst = CAP // 128
    inp('x1', [NB, D])
    inp('h2p', [NB + 128, D], BF16)
    inp('aff_own', [NB, 16])
    inp('aff_all', [128, 32, NA])
    inp('w_gate', [NEXP, D, FF])
    inp('w_up', [NEXP, D, FF])
    inp('w_down', [NEXP, FF, D])
    inp('fgb', [128, D])
    inp('ustrict', [128, 128])
    inp('ident_bf', [128, 128], BF16)
    y_out = nc.dram_tensor('y', [NB, D], F32, kind="ExternalOutput").ap()
    skind = "ExternalOutput" if debug else "Internal"
    x2 = nc.dram_tensor('x2', [NB + 128, D], F32, kind=skind).ap()
    lists = [nc.dram_tensor('lists%d' % e_, [CAPP, 2], F32, kind=skind).ap() for e_ in range(NEXP)]
    thr_dbg = nc.dram_tensor('thr_dbg', [128, 32], F32, kind=skind).ap()

    with contextlib.ExitStack() as es0:
        def sb0(name, shape, dt):
            return es0.enter_context(nc.sbuf_tensor("m_" + name, list(shape), dt))

        banks = [es0.enter_context(nc.psum_tensor("mbk%d" % i, [128, 512], F32)) for i in range(8)]
        ones_f = sb0("ones_f", [128, 128], F32)
        lo = sb0("lo", [128, 32], F32)
        identb = sb0("identb", [128, 128], BF16)
        idxi = sb0("idxi", [128, ntt, 16], I32)
        src = sb0("src", [128, ntt, 16, 2], F32)
        kb.op('pool', lambda e: e.memset(ones_f[:], 1.0), writes=['ones_f'])
        kb.dma('sp', identb[:], io['ident_bf'][:, :], writes=['identb'])
        rows = NB // 8
        for i in range(8):
            kb.dma('sp', x2[i * rows:(i + 1) * rows, :], io['x1'][i * rows:(i + 1) * rows, :], writes=[kb.uq()])

        with contextlib.ExitStack() as es:
            def sb(name, shape, dt):
                return es.enter_context(nc.sbuf_tensor("a_" + name, list(shape), dt))
            A = sb("A", [128, 32, NA], F32)
            cmp_ = sb("cmp", [128, 32, NA], F32)
            hi = sb("hi", [128, 32], F32)
            mid = sb("mid", [128, 32], F32)
            cnt = sb("cnt", [128, 32], F32)
            ge = sb("ge", [128, 32], F32)
            d1 = sb("d1", [128, 32], F32)
            d2 = sb("d2", [128, 32], F32)
            zrow = sb("zrow", [128, D], F32)
            kb.op('pool', lambda e: e.memset(zrow[:], 0.0), writes=['zrow'])
            kb.dma('sp', x2[NB:NB + 128, :], zrow[:], reads=['zrow'], writes=[kb.uq()])
            kb.dma('sp', A[:], io['aff_all'][:, :, :], writes=['A'])
            kb.op('pool', lambda e: e.memset(lo[:], 0.0), writes=['lo'])
            kb.op('pool', lambda e: e.memset(hi[:], 1.0), writes=['hi'])
            for it in range(32):
                kb.op('dve', lambda e: e.tensor_tensor(out=mid[:], in0=lo[:], in1=hi[:], op=ALU.add),
                      reads=['lo', 'hi'], writes=['mid'])
                kb.op('dve', lambda e: e.tensor_scalar(out=mid[:], in0=mid[:], scalar1=0.5, scalar2=None, op0=ALU.mult),
                      reads=['mid'], writes=['mid'])
                for ge_ in range(32):
                    kb.op('dve', lambda e: e.tensor_scalar(out=cmp_[:, ge_, :], in0=A[:, ge_, :],
                                                           scalar1=mid[:, ge_:ge_ + 1], scalar2=0.0, op0=ALU.is_ge,
                                                           op1=ALU.add, accum_out=cnt[:, ge_:ge_ + 1]),
                          reads=['A', 'mid'], writes=[('cmp', ge_), ('cnt', ge_)])
                kb.op('pe', lambda e: e.matmul(banks[0][:, 0:32], lhsT=ones_f[:], rhs=cnt[:], start=True, stop=True),
                      reads=[('cnt', g__) for g__ in range(32)] + ['ones_f'], writes=['mbk0'])
                kb.op('dve', lambda e: e.tensor_single_scalar(out=ge[:], in_=banks[0][:, 0:32],
                                                              scalar=float(c2.topk) - 0.5, op=ALU.is_ge),
                      reads=['mbk0'], writes=['ge'])
                kb.op('dve', lambda e: e.tensor_tensor(out=d1[:], in0=mid[:], in1=lo[:], op=ALU.subtract),
                      reads=['mid', 'lo'], writes=['d1'])
                kb.op('dve', lambda e: e.tensor_tensor(out=d1[:], in0=d1[:], in1=ge[:], op=ALU.mult),
                      reads=['d1', 'ge'], writes=['d1'])
                kb.op('dve', lambda e: e.tensor_tensor(out=d2[:], in0=hi[:], in1=mid[:], op=ALU.subtract),
                      reads=['mid', 'hi'], writes=['d2'])
                kb.op('dve', lambda e: e.tensor_tensor(out=d2[:], in0=d2[:], in1=ge[:], op=ALU.mult),
                      reads=['d2', 'ge'], writes=['d2'])
                kb.op('dve', lambda e: e.tensor_tensor(out=lo[:], in0=lo[:], in1=d1[:], op=ALU.add),
                      reads=['lo', 'd1'], writes=['lo'])
                kb.op('dve', lambda e: e.tensor_tensor(out=hi[:], in0=mid[:], in1=d2[:], op=ALU.add),
                      reads=['mid', 'd2'], writes=['hi'])
            kb.dma('sp', thr_dbg[:, :], lo[:], reads=['lo'], writes=['thr_dbg'])
            kb.barrier()

        with contextlib.ExitStack() as es:
            def sb(name, shape, dt):
                return es.enter_context(nc.sbuf_tensor("b_" + name, list(shape), dt))
            NC = ntt * 16
            affo = sb("affo", [128, ntt, 16], F32)
            sel = sb("sel", [128, ntt, 16], F32)
            within = sb("within", [128, ntt, 16], F32)
            cntb = sb("cntb", [128, ntt, 16], F32)
            incl = sb("incl", [128, ntt, 16], F32)
            zer = sb("zer", [128, ntt], F32)
            idxf = sb("idxf", [128, ntt, 16], F32)
            tokf = sb("tokf", [128, ntt], F32)
            ust = sb("ust", [128, 128], F32)
            fill = sb("fill", [128, CAPP // 128, 2], F32)
            kb.dma('sp', ust[:], io['ustrict'][:, :], writes=['ust'])
            kb.dma('sp', affo[:], io['aff_own'].rearrange("(t p) e -> p t e", p=128), writes=['affo'])
            kb.op('pool', lambda e: e.memset(zer[:], 0.0), writes=['zer'])
            JF = CAPP // 128
            filli = sb("filli", [128, JF], I32)
            pio = sb("pio", [128, 1], F32)
            kb.op('pool', lambda e: e.iota(filli[:], pattern=[[1, JF]], base=0, channel_multiplier=JF), writes=['filli'])
            kb.op('pool', lambda e: e.iota(pio[:], pattern=[[0, 1]], base=CAP, channel_multiplier=1,
                                           allow_small_or_imprecise_dtypes=True), writes=['pio'])
            kb.op('dve', lambda e: e.tensor_single_scalar(out=filli[:], in_=filli[:], scalar=127, op=ALU.bitwise_and),
                  reads=['filli'], writes=['filli'])
            kb.op('dve', lambda e: e.tensor_copy(out=fill[:, :, 0], in_=filli[:]), reads=['filli'], writes=['fill'])
            kb.op('dve', lambda e: e.tensor_scalar(out=fill[:, :, 0], in0=fill[:, :, 0], scalar1=float(NB), scalar2=None,
                                                   op0=ALU.add), reads=['fill'], writes=['fill'])
            kb.op('pool', lambda e: e.memset(fill[:, :, 1:2], 0.0), reads=['fill'], writes=['fill'])
            for ex in range(NEXP):
                kb.dma('sp', lists[ex].rearrange("(p j) c -> p j c", p=128), fill[:], reads=['fill'], writes=[('lists', ex)])
            kb.op('pool', lambda e: e.iota(tokf[:], pattern=[[128, ntt]], base=0, channel_multiplier=1,
                                           allow_small_or_imprecise_dtypes=True), writes=['tokf'])
            for g, (t0, t1) in enumerate(c2.group_tiles):
                kb.op('dve', lambda e: e.tensor_tensor(
                    out=sel[:, t0:t1, :], in0=affo[:, t0:t1, :],
                    in1=lo[:, g * 16:(g + 1) * 16].unsqueeze(1).to_broadcast([128, t1 - t0, 16]), op=ALU.is_ge),
                    reads=['affo', 'lo'], writes=['sel'])

            def fl(t):
                return t[:].rearrange("p t e -> p (t e)")
            for c0 in range(0, NC, 512):
                cw = min(512, NC - c0)
                kb.op('pe', lambda e: e.matmul(banks[1][:, 0:cw], lhsT=ust[:], rhs=fl(sel)[:, c0:c0 + cw],
                                               start=True, stop=True), reads=['sel', 'ust'], writes=['mbk1'])
                kb.op('dve', lambda e: e.tensor_copy(out=fl(within)[:, c0:c0 + cw], in_=banks[1][:, 0:cw]),
                      reads=['mbk1'], writes=['within'])
                kb.op('pe', lambda e: e.matmul(banks[2][:, 0:cw], lhsT=ones_f[:], rhs=fl(sel)[:, c0:c0 + cw],
                                               start=True, stop=True), reads=['sel', 'ones_f'], writes=['mbk2'])
                kb.op('dve', lambda e: e.tensor_copy(out=fl(cntb)[:, c0:c0 + cw], in_=banks[2][:, 0:cw]),
                      reads=['mbk2'], writes=['cntb'])
            for ex in range(16):
                kb.op('dve', lambda e: e.tensor_tensor_scan(out=incl[:, :, ex], data0=cntb[:, :, ex], data1=zer[:],
                                                            initial=0.0, op0=ALU.add, op1=ALU.add),
                      reads=['cntb', 'zer'], writes=['incl'])
            kb.op('dve', lambda e: e.tensor_tensor(out=fl(idxf), in0=fl(within), in1=fl(incl), op=ALU.add),
                  reads=['within', 'incl'], writes=['idxf'])
            kb.op('dve', lambda e: e.tensor_tensor(out=fl(idxf), in0=fl(idxf), in1=fl(cntb), op=ALU.subtract),
                  reads=['idxf', 'cntb'], writes=['idxf'])
            kb.op('dve', lambda e: e.tensor_single_scalar(out=fl(incl), in_=fl(idxf), scalar=float(CAP) - 0.5, op=ALU.is_lt),
                  reads=['idxf'], writes=['incl'])
            kb.op('dve', lambda e: e.tensor_tensor(out=fl(incl), in0=fl(incl), in1=fl(sel), op=ALU.mult),
                  reads=['incl', 'sel'], writes=['incl'])
            kb.op('dve', lambda e: e.tensor_scalar(out=fl(idxf), in0=fl(idxf), scalar1=pio[:, 0:1], scalar2=None,
                                                   op0=ALU.subtract), reads=['idxf', 'pio'], writes=['idxf'])
            kb.op('dve', lambda e: e.tensor_tensor(out=fl(idxf), in0=fl(idxf), in1=fl(incl), op=ALU.mult),
                  reads=['idxf', 'incl'], writes=['idxf'])
            kb.op('dve', lambda e: e.tensor_scalar(out=fl(idxf), in0=fl(idxf), scalar1=pio[:, 0:1], scalar2=None,
                                                   op0=ALU.add), reads=['idxf', 'pio'], writes=['idxf'])
            kb.op('dve', lambda e: e.tensor_copy(out=fl(idxi), in_=fl(idxf)), reads=['idxf'], writes=['idxi'])
            kb.op('pool', lambda e: e.tensor_copy(out=src[:, :, :, 0], in_=tokf[:].unsqueeze(2).to_broadcast([128, ntt, 16])),
                  reads=['tokf'], writes=['src'])
            kb.op('pool', lambda e: e.tensor_copy(out=src[:, :, :, 1], in_=affo[:]), reads=['affo', 'src'], writes=['src'])
            kb.barrier()

        def scat(ex, tt):
            kb._dma_like('pool', lambda e: e.indirect_dma_start(
                out=lists[ex][:, :], out_offset=bass.IndirectOffsetOnAxis(ap=idxi[:, tt, ex:ex + 1], axis=0),
                in_=src[:, tt, ex, :], in_offset=None), reads=[], writes=[('lists', ex)])

        def scat_gen(ex):
            for tt in range(ntt):
                scat(ex, tt)
                yield

        for tt in range(ntt):
            scat(0, tt)
            if NEXP > 1:
                scat(1, tt)

        with contextlib.ExitStack() as es:
            def sb(name, shape, dt):
                return es.enter_context(nc.sbuf_tensor("c_" + name, list(shape), dt))
            hT = sb("hT", [128, NFT, CAP], BF16)
            wd = sb("wd", [128, NFT, D], BF16)
            xeT = sb("xeT", [128, 8, CAP], BF16)
            lsts = Rot([sb("lst%d" % i, [128, nst, 2], F32) for i in range(2)])
            tokis = Rot([sb("toki%d" % i, [128, nst], I32) for i in range(2)])
            xes = Rot([sb("xe%d" % i, [128, D], BF16) for i in range(3)])
            wgst = Rot([sb("wgst%d" % i, [128, 8, 128], F32) for i in range(2)])
            wust = Rot([sb("wust%d" % i, [128, 8, 128], F32) for i in range(2)])
            wdst = Rot([sb("wdst%d" % i, [128, D], F32) for i in range(2)])
            wgbs = Rot([sb("wgb%d" % i, [128, 8, 128], BF16) for i in range(2)])
            wubs = Rot([sb("wub%d" % i, [128, 8, 128], BF16) for i in range(2)])
            sils = Rot([sb("sil%d" % i, [128, 512], F32) for i in range(2)])
            ysbs = Rot([sb("ysb%d" % i, [128, D], F32) for i in range(2)])
            kb.op('pool', lambda e: e.memset(xeT[:], 0.0), writes=['xeT'])
            sgs = [(c0, min(512, CAP - c0)) for c0 in range(0, CAP, 512)]
            for ex in range(NEXP):
                lst = lsts.next(); toki = tokis.next()
                kb.dma('sp', lst[:], lists[ex][0:CAP, :].rearrange("(j p) c -> p j c", p=128),
                       reads=[('lists', ex)], writes=[lst.name])
                kb.op('dve', lambda e: e.tensor_copy(out=toki[:], in_=lst[:, :, 0]), reads=[lst.name], writes=[toki.name])
                for j in range(nst):
                    xe = xes.next()
                    kb._dma_like('pool', lambda e: e.indirect_dma_start(
                        out=xe[:], out_offset=None, in_=io['h2p'][:, :],
                        in_offset=bass.IndirectOffsetOnAxis(ap=toki[:, j:j + 1], axis=0)),
                        reads=[toki.name], writes=[xe.name])
                    bk = banks[j % 2]
                    bkb = bk[:].bitcast(BF16)
                    for k in range(8):
                        kb.op('pe', lambda e: e.transpose(out=bkb[:, k * 128:(k + 1) * 128],
                                                          in_=xe[:, k * 128:(k + 1) * 128], identity=identb[:]),
                              reads=[xe.name, 'identb'], writes=[bk.name])
                    kb.op('dve', lambda e: e.tensor_copy(out=xeT[:, :, j * 128:(j + 1) * 128],
                                                         in_=bkb[:, 0:1024].rearrange("p (k t) -> p k t", k=8)),
                          reads=[bk.name], writes=['xeT'])
                sg_ = scat_gen(ex + 2) if ex + 2 < NEXP else None
                per_ft = -(-ntt // NFT)
                for ft in range(NFT):
                    if sg_ is not None:
                        for _ in range(per_ft):
                            try:
                                next(sg_)
                            except StopIteration:
                                sg_ = None
                                break
                    wg_s = wgst.next(); wu_s = wust.next(); wd_s = wdst.next(); wgb = wgbs.next(); wub = wubs.next()
                    fs = slice(ft * 128, (ft + 1) * 128)
                    for hh in range(2):
                        ks = slice(hh * 4, (hh + 1) * 4)
                        kb.dma('sp', wg_s[:, ks, :],
                               io['w_gate'][ex, hh * 512:(hh + 1) * 512, fs].rearrange("(k p) f -> p k f", p=128),
                               writes=[(wg_s.name, hh)])
                        kb.dma('sp', wu_s[:, ks, :],
                               io['w_up'][ex, hh * 512:(hh + 1) * 512, fs].rearrange("(k p) f -> p k f", p=128),
                               writes=[(wu_s.name, hh)])
                    kb.dma('sp', wd_s[:], io['w_down'][ex, fs, :], writes=[wd_s.name])
                    kb.op('dve', lambda e: e.tensor_copy(out=wgb[:], in_=wg_s[:]), reads=[(wg_s.name, 0), (wg_s.name, 1)], writes=[wgb.name])
                    kb.op('dve', lambda e: e.tensor_copy(out=wub[:], in_=wu_s[:]), reads=[(wu_s.name, 0), (wu_s.name, 1)], writes=[wub.name])
                    kb.op('act', lambda e: e.copy(out=wd[:, ft, :], in_=wd_s[:]), reads=[wd_s.name],
                          writes=[('wd', ft)])
                    for gi, (c0, cw) in enumerate(sgs):
                        ba = banks[2 + gi % 2]
                        bbk = banks[4 + gi % 2]
                        sil = sils.next()
                        for k in range(8):
                            kb.op('pe', lambda e: e.matmul(ba[:, 0:cw], lhsT=wgb[:, k, :], rhs=xeT[:, k, c0:c0 + cw],
                                                           start=(k == 0), stop=(k == 7)),
                                  reads=[wgb.name, 'xeT'], writes=[ba.name])
                        for k in range(8):
                            kb.op('pe', lambda e: e.matmul(bbk[:, 0:cw], lhsT=wub[:, k, :], rhs=xeT[:, k, c0:c0 + cw],
                                                           start=(k == 0), stop=(k == 7)),
                                  reads=[wub.name, 'xeT'], writes=[bbk.name])
                        kb.op('act', lambda e: e.activation(out=sil[:, 0:cw], in_=ba[:, 0:cw], func=AF.Silu),
                              reads=[ba.name], writes=[sil.name])
                        kb.op('dve', lambda e: e.tensor_tensor(out=hT[:, ft, c0:c0 + cw], in0=sil[:, 0:cw],
                                                               in1=bbk[:, 0:cw], op=ALU.mult),
                              reads=[sil.name, bbk.name], writes=[('hT', ft)])
                if sg_ is not None:
                    for _ in sg_:
                        pass
                hkeys = [('hT', ft) for ft in range(NFT)]
                wkeys = [('wd', ft) for ft in range(NFT)]
                for j in range(nst):
                    ysb = ysbs.next()
                    for hf in range(2):
                        bk = banks[6 + hf]
                        for ft in range(NFT):
                            kb.op('pe', lambda e: e.matmul(bk[:, :], lhsT=hT[:, ft, j * 128:(j + 1) * 128],
                                                           rhs=wd[:, ft, hf * 512:(hf + 1) * 512],
                                                           start=(ft == 0), stop=(ft == NFT - 1)),
                                  reads=[hkeys[ft], wkeys[ft]], writes=[bk.name])
                        kb.op('act', lambda e: e.activation(out=ysb[:, hf * 512:(hf + 1) * 512], in_=bk[:, :],
                                                            func=AF.Copy, scale=lst[:, j, 1:2]),
                              reads=[bk.name, lst.name], writes=[(ysb.name, hf)])
                    kb._dma_like('pool', lambda e: e.indirect_dma_start(
                        out=x2[:, :], out_offset=bass.IndirectOffsetOnAxis(ap=toki[:, j:j + 1], axis=0),
                        in_=ysb[:], in_offset=None, compute_op=ALU.add),
                        reads=[(ysb.name, 0), (ysb.name, 1), toki.name, 'x2'], writes=['x2'])
            kb.barrier()

        with contextlib.ExitStack() as es:
            def sb(name, shape, dt):
                return es.enter_context(nc.sbuf_tensor("f_" + name, list(shape), dt))
            fgb = sb("fgb", [128, D], F32)
            xts = Rot([sb("xt%d" % i, [128, D], F32) for i in range(3)])
            yts = Rot([sb("yt%d" % i, [128, D], F32) for i in range(3)])
            sss = Rot([sb("ss%d" % i, [128, 4], F32) for i in range(3)])
            junk = sb("junk", [128, D], BF16)
            kb.dma('sp', fgb[:], io['fgb'][:, :], writes=['fgb'])
            for tt in range(ntt):
                xt = xts.next(); yt = yts.next(); ss = sss.next()
                kb.dma('sp', xt[:], x2[tt * 128:(tt + 1) * 128, :], reads=['x2'], writes=[xt.name])
                kb.op('act', lambda e: e.activation(out=junk[:], in_=xt[:], func=AF.Square, accum_out=ss[:, 0:1]),
                      reads=[xt.name], writes=['junk', ss.name])
                kb.op('act', lambda e: e.activation(out=ss[:, 1:2], in_=ss[:, 0:1], func=AF.Sqrt, scale=1.0 / D, bias=EPS),
                      reads=[ss.name], writes=[ss.name])
                kb.op('dve', lambda e: e.reciprocal(out=ss[:, 2:3], in_=ss[:, 1:2]), reads=[ss.name], writes=[ss.name])
                kb.op('dve', lambda e: e.scalar_tensor_tensor(out=yt[:], in0=xt[:], scalar=ss[:, 2:3], in1=fgb[:],
                                                              op0=ALU.mult, op1=ALU.mult),
                      reads=[xt.name, ss.name, 'fgb'], writes=[yt.name])
                kb.dma('sp', y_out[tt * 128:(tt + 1) * 128, :], yt[:], reads=[yt.name], writes=['y_out'])
            kb.barrier()
    return nc, kb


def _t5_bucket_np(rel):
    import math
    nb = 16
    ret = np.where(rel > 0, nb, 0)
    n = np.abs(rel)
    max_exact = nb // 2
    nf = np.maximum(n, 1).astype(np.float32)
    large = max_exact + (np.log(nf / np.float32(max_exact)) / np.float32(math.log(1024 / max_exact))
                         * np.float32(nb - max_exact)).astype(np.int32)
    large = np.minimum(large, nb - 1)
    return ret + np.where(n < max_exact, n, large)


def make_biasT(rel_bias):
    j = np.arange(128)[:, None, None]
    kt = np.arange(2)[None, :, None]
    i = np.arange(128)[None, None, :]
    delta = -64 + 128 * kt + j - i
    table = np.concatenate([np.asarray(rel_bias, np.float32), np.full((1, 8), -30000.0, np.float32)], axis=0)
    out = np.zeros((128, 3, 8, 2, 128), np.float32)
    for b, d in enumerate(DILS):
        idx = _t5_bucket_np(delta * d)
        idx = np.where(np.abs(delta) <= 64, idx, 32)
        out[:, b] = np.transpose(table[idx], (0, 3, 1, 2))
    return np.ascontiguousarray(out.reshape(128, 24, 256))


def build_program(cfg, debug=False):
    nc = bass.Bass("TRN2", target_bir_lowering=False)
    kb = KB(nc)
    io = {}

    def inp(name, shape, dt=F32):
        io[name] = nc.dram_tensor(name, list(shape), dt, kind="ExternalInput").ap()

    inp('xs', [cfg.NACT, D])
    inp('valid', [cfg.NACT, 1])
    inp('w_in', [D, PW])
    inp('g1', [128, 8])
    inp('ident_bf', [128, 128], BF16)
    skind = "ExternalOutput" if debug else "Internal"
    scr = {}

    def scratch(name, shape, dt):
        scr[name] = nc.dram_tensor(name, list(shape), dt, kind=skind).ap()

    scratch('qkT', [1024, cfg.NTS], BF16)
    scratch('vaug', [cfg.NTS, 1024], BF16)
    scratch('dnT', [1536, cfg.NTS], F32)
    scratch('zs', [cfg.NTS, 512], F32)
    scratch('sc', [cfg.NTS, 16], F32)
    scratch('mixT', [1024, cfg.NBODY], BF16)
    inp('biasT', [128, 24, 256])
    if not getattr(cfg, 'skip_1a', False):
        phase1a(kb, cfg, io, scr)
    inp('cmask', [128, 6, 128])
    inp('dnc', [128, 16])
    inp('convw', [128, 12, 3])
    inp('gn', [128, 128])
    if not getattr(cfg, 'skip_1c', False):
        phase1c(kb, cfg, io, scr)
    if not getattr(cfg, 'skip_1b', False):
        phase1b(kb, cfg, io, scr)
    inp('g2b', [128, D])
    inp('ident_f', [128, 128])
    inp('w_router', [D, 16])
    inp('w_out', [D, D])
    outs = {}
    outs['x1'] = nc.dram_tensor('x1', [cfg.NBODY, D], F32, kind="ExternalOutput").ap()
    outs['h2'] = nc.dram_tensor('h2', [cfg.NBODY, D], BF16, kind="ExternalOutput").ap()
    outs['aff'] = nc.dram_tensor('aff', [cfg.NBODY, 16], F32, kind="ExternalOutput").ap()
    if not getattr(cfg, 'skip_1d', False):
        phase1d(kb, cfg, io, scr, outs)
    return nc, kb


_PROGS = {}


def _get_progs():
    if 'p1' not in _PROGS:
        _PROGS['p1'] = build_program(FULL_CFG)[0]
        _PROGS['p2'] = build_program2(FULL_CFG2)[0]
    return _PROGS['p1'], _PROGS['p2']


def kernel(x_prompt, x_sample, rel_bias, norm1_g, w_in, conv_w, a_log_fwd, dt_bias_fwd, a_log_bwd,
           dt_bias_bwd, dn_norm_g, w_out, norm2_g, w_router, w_gate, w_up, w_down, final_norm_g):
    f32 = np.float32
    x_prompt = np.asarray(x_prompt, f32)
    x_sample = np.asarray(x_sample, f32)
    nc1, nc2 = _get_progs()
    cfg = FULL_CFG
    ident_bf = np.eye(128).astype(ml_dtypes.bfloat16)
    shared1 = dict(
        w_in=np.ascontiguousarray(np.asarray(w_in, f32)[0]),
        g1=np.ascontiguousarray(np.asarray(norm1_g, f32)[0].reshape(8, 128).T),
        ident_bf=ident_bf,
        biasT=make_biasT(np.asarray(rel_bias, f32)),
        cmask=make_cmask(),
        dnc=np.ascontiguousarray(np.tile(np.concatenate([np.asarray(dt_bias_fwd, f32)[0], np.asarray(dt_bias_bwd, f32)[0],
                                                         np.asarray(a_log_fwd, f32)[0], np.asarray(a_log_bwd, f32)[0]])[None],
                                         (128, 1))),
        convw=np.ascontiguousarray(np.asarray(conv_w, f32)[0].reshape(3, 12, 128).transpose(2, 1, 0)),
        gn=np.ascontiguousarray(np.tile(np.asarray(dn_norm_g, f32)[0][None], (128, 1))),
        g2b=np.ascontiguousarray(np.tile(np.asarray(norm2_g, f32)[0][None], (128, 1))),
        ident_f=np.eye(128, dtype=f32),
        w_router=np.ascontiguousarray(np.asarray(w_router, f32)[0]),
        w_out=np.ascontiguousarray(np.asarray(w_out, f32)[0]),
    )
    SQ = 4096
    in_maps = []
    for c in range(NCORES):
        sidx, qd = c // 4, c % 4
        seg = np.zeros((SQ + 2 * H, D), f32)
        val = np.zeros((SQ + 2 * H, 1), f32)
        lo = qd * SQ - H
        hi = qd * SQ + SQ + H
        a, b = max(lo, 0), min(hi, x_sample.shape[1])
        seg[a - lo:b - lo] = x_sample[sidx, a:b]
        val[a - lo:b - lo] = 1.0
        xs = np.concatenate([x_prompt[2 * c], x_prompt[2 * c + 1], seg], axis=0)
        valid = np.concatenate([np.ones((4096, 1), f32), val], axis=0)
        m = dict(shared1)
        m['xs'] = np.ascontiguousarray(xs)
        m['valid'] = valid
        in_maps.append(m)
    r1 = run_bass_kernel_spmd(nc1, in_maps, core_ids=list(range(NCORES))).results
    aff_all = np.zeros((128, 32, 256), f32)
    for g in range(2):
        a = np.concatenate([np.asarray(r1[c]['aff'])[g * 4096:(g + 1) * 4096] for c in range(NCORES)], axis=0)
        aff_all[:, g * 16:(g + 1) * 16, :] = a.reshape(256, 128, 16).transpose(1, 2, 0)
    shared2 = dict(
        aff_all=aff_all,
        w_gate=np.ascontiguousarray(np.asarray(w_gate, f32)[0]),
        w_up=np.ascontiguousarray(np.asarray(w_up, f32)[0]),
        w_down=np.ascontiguousarray(np.asarray(w_down, f32)[0]),
        fgb=np.ascontiguousarray(np.tile(np.asarray(final_norm_g, f32)[None], (128, 1))),
        ustrict=(np.arange(128)[:, None] < np.arange(128)[None, :]).astype(f32),
        ident_bf=ident_bf,
    )
    in_maps2 = []
    for c in range(NCORES):
        m = dict(shared2)
        m['x1'] = np.asarray(r1[c]['x1'])
        m['h2p'] = np.concatenate([np.asarray(r1[c]['h2']), np.zeros((128, D), ml_dtypes.bfloat16)], axis=0)
        m['aff_own'] = np.asarray(r1[c]['aff'])
        in_maps2.append(m)
    r2 = run_bass_kernel_spmd(nc2, in_maps2, core_ids=list(range(NCORES))).results
    y_prompt = np.zeros(x_prompt.shape, f32)
    y_sample = np.zeros(x_sample.shape, f32)
    for c in range(NCORES):
        y = np.asarray(r2[c]['y'])
        y_prompt[2 * c] = y[0:2048]
        y_prompt[2 * c + 1] = y[2048:4096]
        y_sample[c // 4, (c % 4) * SQ:(c % 4 + 1) * SQ] = y[4096:8192]
    return (y_prompt, y_sample)
```

```python
import contextlib
import numpy as np
import ml_dtypes
import concourse.bass as bass
import concourse.mybir as mybir
from concourse.bass_utils import run_bass_kernel_spmd

F32 = mybir.dt.float32
BF16 = mybir.dt.bfloat16
I32 = mybir.dt.int32
U32 = mybir.dt.uint32
AF = mybir.ActivationFunctionType
ALU = mybir.AluOpType
AX = mybir.AxisListType

H = 1024
D = 1024
PW = 3600
EPS = 1e-6
NCORES = 8


class KB:
    ND = 16

    def __init__(self, nc):
        self.nc = nc
        self.E = {'pe': nc.tensor, 'dve': nc.vector, 'act': nc.scalar,
                  'pool': nc.gpsimd, 'sp': nc.sync}
        self.sems = {}
        self.cnt = {}
        for e in self.E:
            self.sems[e] = nc.semaphore('s_' + e).__enter__()
            self.cnt[e] = 0
        self.seen = {e: {} for e in self.E}
        self.dcnt = {}
        self.dnext = {}
        for q in ('sp', 'act', 'pool'):
            self.dnext[q] = 0
            for i in range(self.ND):
                k = ('d', q, i)
                self.sems[k] = nc.semaphore('d_%s_%d' % (q, i)).__enter__()
                self.dcnt[k] = 0
        self.lastw = {}
        self.readers = {}
        self.ninst = 0

    def wait(self, eng, tok):
        k, v = tok
        if self.seen[eng].get(k, 0) >= v:
            return
        if k == eng and eng == 'pe':
            return
        self.E[eng].wait_ge(self.sems[k], v)
        self.seen[eng][k] = v
        self.ninst += 1

    def uq(self):
        self._u = getattr(self, '_u', 0) + 1
        return ('uq', self._u)

    def _deps(self, eng, reads, writes, war=()):
        for b in war:
            for t in list(self.readers.get(b, {}).items()):
                self.wait(eng, t)
        for b in list(reads) + list(writes):
            t = self.lastw.get(b)
            if t is not None:
                self.wait(eng, t)
        for b in writes:
            for t in list(self.readers.get(b, {}).items()):
                self.wait(eng, t)

    def _commit(self, tok, reads, writes):
        for b in writes:
            self.lastw[b] = tok
            self.readers[b] = {}
        for b in reads:
            r = self.readers.setdefault(b, {})
            if r.get(tok[0], 0) < tok[1]:
                r[tok[0]] = tok[1]

    def op(self, eng, fn, reads=(), writes=()):
        self._deps(eng, reads, writes)
        inst = fn(self.E[eng])
        self.cnt[eng] += 1
        inst.then_inc(self.sems[eng], 1)
        tok = (eng, self.cnt[eng])
        self.ninst += 1
        self._commit(tok, reads, writes)
        return tok

    def _dma_like(self, q, fn, reads, writes, war=()):
        slot = self.dnext[q]
        self.dnext[q] = (slot + 1) % self.ND
        k = ('d', q, slot)
        if self.dcnt[k] > 0:
            self.wait(q, (k, 16 * self.dcnt[k]))
        self._deps(q, reads, writes, war)
        inst = fn(self.E[q])
        self.dcnt[k] += 1
        inst.then_inc(self.sems[k], 16)
        tok = (k, 16 * self.dcnt[k])
        self.ninst += 1
        self._commit(tok, reads, writes)
        return tok

    def dma(self, q, out, in_, reads=(), writes=(), war=(), **kw):
        return self._dma_like(q, lambda e: e.dma_start(out=out, in_=in_, **kw), reads, writes, war)

    def barrier(self):
        for eng in self.E:
            for k, c in self.dcnt.items():
                if c > 0:
                    self.wait(eng, (k, 16 * c))
            for e in self.E:
                if e != eng and self.cnt[e] > 0:
                    self.wait(eng, (e, self.cnt[e]))
        self.lastw = {}
        self.readers = {}


class Rot:
    def __init__(self, items):
        self.items = items
        self.i = 0

    def next(self):
        t = self.items[self.i % len(self.items)]
        self.i += 1
        return t


class Cfg:
    def __init__(self, segs):
        self.segs = segs
        self.slot_base = []
        self.act_base = []
        b = a = 0
        for body, full in segs:
            self.slot_base.append(b)
            self.act_base.append(a)
            b += body + 2 * H
            a += (body + 2 * H) if full else body
        self.NTS = b
        self.NBODY = sum(bd for bd, _ in segs)
        self.NACT = a


FULL_CFG = Cfg([(2048, False), (2048, False), (4096, True)])


def phase1a(kb, cfg, io, scr):
    nc = kb.nc
    with contextlib.ExitStack() as es:
        def sb(name, shape, dt):
            return es.enter_context(nc.sbuf_tensor(name, list(shape), dt))

        def ps(name, shape, dt=F32):
            return es.enter_context(nc.psum_tensor(name, list(shape), dt))

        w_sb = sb("w_sb", [128, 8, PW], BF16)
        wst = [sb("wst%d" % i, [128, PW], F32) for i in range(2)]
        g_sb = sb("g1_sb", [128, 8], F32)
        ident = sb("ident", [128, 128], BF16)
        zero_t = sb("zero_t", [128, 4096], BF16)
        xts = Rot([sb("xt%d" % i, [128, D], F32) for i in range(3)])
        xns = Rot([sb("xn%d" % i, [128, D], BF16) for i in range(2)])
        junk = sb("junk", [128, D], BF16)
        sss = Rot([sb("ss%d" % i, [128, 4], F32) for i in range(3)])
        vals = Rot([sb("val%d" % i, [128, 1], F32) for i in range(8)])
        hTs = Rot([sb("hT%d" % i, [128, 8, 512], BF16) for i in range(2)])
        st32 = Rot([sb("st32_%d" % i, [128, 512], F32) for i in range(4)])
        st16 = Rot([sb("st16_%d" % i, [128, 512], BF16) for i in range(3)])
        stv = Rot([sb("stv%d" % i, [128, 4, 2, 128], BF16) for i in range(2)])
        stsc = Rot([sb("stsc%d" % i, [128, 16], F32) for i in range(2)])
        tps = Rot([ps("tp%d" % i, [128, 8, 128], BF16) for i in range(2)])
        pps = Rot([ps("pp%d" % i, [128, 512], F32) for i in range(4)])

        kb.dma('sp', ident[:], io['ident_bf'][:, :], writes=['ident'])
        kb.dma('sp', g_sb[:], io['g1'][:, :], writes=['g1'])
        kb.op('pool', lambda e: e.memset(zero_t[:], 0.0), writes=['zero_t'])
        for k in range(8):
            w = wst[k % 2]
            kb.dma('sp', w[:], io['w_in'][k * 128:(k + 1) * 128, :], writes=[w.name])
            kb.op('dve' if k % 2 == 0 else 'pool',
                  lambda e, w=w, k=k: e.tensor_scalar(out=w_sb[:, k, :], in0=w[:], scalar1=g_sb[:, k:k + 1],
                                                      scalar2=None, op0=ALU.mult),
                  reads=[w.name, 'g1'], writes=[('w_sb', k)])
        for s, (body, full) in enumerate(cfg.segs):
            if full:
                continue
            for side in range(0 if 'zero' in getattr(cfg, 'p1a_skip', ()) else 2):
                s0 = cfg.slot_base[s] + (0 if side == 0 else H + body)
                for r in range(4):
                    kb.dma('pool', scr['qkT'][512 + r * 128:512 + (r + 1) * 128, s0:s0 + H], zero_t[:, 0:H],
                           reads=['zero_t'], writes=[kb.uq()])
                for hh in range(2):
                    kb.dma('pool', scr['vaug'][s0 + hh * 512:s0 + (hh + 1) * 512, :].rearrange("(p j) c -> p j c", j=4),
                           zero_t[:].rearrange("p (j c) -> p j c", j=4), reads=['zero_t'], writes=[kb.uq()])

        evi = [0]

        def evac(out_ap, in_ap, reads, writes, eng=None):
            if eng is None:
                eng = 'act' if evi[0] % 2 == 0 else 'dve'
                evi[0] += 1
            if eng == 'act':
                return kb.op('act', lambda e: e.copy(out=out_ap, in_=in_ap), reads=reads, writes=writes)
            return kb.op('dve', lambda e: e.tensor_copy(out=out_ap, in_=in_ap), reads=reads, writes=writes)

        wkeys = [('w_sb', k) for k in range(8)]
        for s, (body, full) in enumerate(cfg.segs):
            nact = (body + 2 * H) if full else body
            for g in range(nact // 512):
                a0 = cfg.act_base[s] + g * 512
                s0 = cfg.slot_base[s] + (0 if full else H) + g * 512
                hT = hTs.next()
                vtiles = []
                for j in range(4):
                    xt = xts.next()
                    xn = xns.next()
                    ss = sss.next()
                    val = vals.next()
                    tp = tps.next()
                    vtiles.append(val)
                    r0 = a0 + j * 128
                    kb.dma('sp', xt[:], io['xs'][r0:r0 + 128, :], writes=[xt.name])
                    kb.dma('sp', val[:], io['valid'][r0:r0 + 128, :], writes=[val.name])
                    kb.op('act', lambda e: e.activation(out=junk[:], in_=xt[:], func=AF.Square,
                                                        accum_out=ss[:, 0:1]),
                          reads=[xt.name], writes=['junk', ss.name])
                    kb.op('act', lambda e: e.activation(out=ss[:, 1:2], in_=ss[:, 0:1], func=AF.Sqrt,
                                                        scale=1.0 / D, bias=EPS),
                          reads=[ss.name], writes=[ss.name])
                    kb.op('dve', lambda e: e.reciprocal(out=ss[:, 2:3], in_=ss[:, 1:2]),
                          reads=[ss.name], writes=[ss.name])
                    kb.op('dve', lambda e: e.tensor_scalar(out=xn[:], in0=xt[:], scalar1=ss[:, 2:3],
                                                           scalar2=None, op0=ALU.mult),
                          reads=[xt.name, ss.name], writes=[xn.name])
                    for k in range(8):
                        kb.op('pe', lambda e, k=k: e.transpose(out=tp[:, k, :], in_=xn[:, k * 128:(k + 1) * 128],
                                                               identity=ident[:]),
                              reads=[xn.name, 'ident'], writes=[tp.name])
                    evac(hT[:, :, j * 128:(j + 1) * 128], tp[:], [tp.name], [(hT.name, j)])
                hkeys = [(hT.name, j) for j in range(4)]
                fm_tiles = [(c * 128, 'dn', c) for c in range(12)] + [(2064 + c * 128, 'qk', c) for c in range(8)]
                g_lo, g_hi = g * 512, (g + 1) * 512
                need_dn = (not full) or (g_hi > H - 256 - 1 and g_lo < H + body + 256 + 1)
                need_body = (not full) or (g_hi > H and g_lo < H + body)
                if not need_dn:
                    fm_tiles = [t_ for t_ in fm_tiles if t_[1] != 'dn']
                if not need_body:
                    fm_tiles = [t_ for t_ in fm_tiles if not (t_[1] == 'qk' and t_[2] < 4)]
                for c0, kind, ci in fm_tiles:
                    pp = pps.next()
                    for k in range(8):
                        kb.op('pe', lambda e, k=k: e.matmul(pp[:, :], lhsT=w_sb[:, k, c0:c0 + 128], rhs=hT[:, k, :],
                                                            start=(k == 0), stop=(k == 7)),
                              reads=hkeys + [wkeys[k]], writes=[pp.name])
                    if kind == 'dn':
                        st = st32.next()
                        evac(st[:], pp[:], [pp.name], [st.name])
                        kb.dma('pool', scr['dnT'][ci * 128:(ci + 1) * 128, s0:s0 + 512], st[:],
                               reads=[st.name], writes=[kb.uq()])
                    else:
                        st = st16.next()
                        evac(st[:], pp[:], [pp.name], [st.name])
                        kb.dma('pool', scr['qkT'][ci * 128:(ci + 1) * 128, s0:s0 + 512], st[:],
                               reads=[st.name], writes=[kb.uq()])
                for j in range(4):
                    t0 = s0 + j * 128
                    if need_body:
                        pp = pps.next()
                        for k in range(8):
                            kb.op('pe', lambda e, k=k: e.matmul(pp[:, :], lhsT=hT[:, k, j * 128:(j + 1) * 128],
                                                                rhs=w_sb[:, k, 1536:2048], start=(k == 0), stop=(k == 7)),
                                  reads=hkeys + [wkeys[k]], writes=[pp.name])
                        st = st32.next()
                        evac(st[:], pp[:], [pp.name], [st.name])
                        kb.dma('pool', scr['zs'][t0:t0 + 128, :], st[:], reads=[st.name], writes=[kb.uq()])
                    if need_dn:
                        pp = pps.next()
                        for k in range(8):
                            kb.op('pe', lambda e, k=k: e.matmul(pp[:, 0:16], lhsT=hT[:, k, j * 128:(j + 1) * 128],
                                                                rhs=w_sb[:, k, 2048:2064], start=(k == 0), stop=(k == 7)),
                                  reads=hkeys + [wkeys[k]], writes=[pp.name])
                        stc = stsc.next()
                        evac(stc[:], pp[:, 0:16], [pp.name], [stc.name])
                        kb.dma('pool', scr['sc'][t0:t0 + 128, :], stc[:], reads=[stc.name], writes=[kb.uq()])
                    if 'v' in getattr(cfg, 'p1a_skip', ()):
                        continue
                    pp = pps.next()
                    for k in range(8):
                        kb.op('pe', lambda e, k=k: e.matmul(pp[:, :], lhsT=hT[:, k, j * 128:(j + 1) * 128],
                                                            rhs=w_sb[:, k, 3088:3600], start=(k == 0), stop=(k == 7)),
                              reads=hkeys + [wkeys[k]], writes=[pp.name])
                    sv = stv.next()
                    val = vtiles[j]
                    ppv = pp[:].rearrange("p (h t c) -> p h t c", h=4, t=2)
                    evac(sv[:, :, 0, 0:64], ppv[:, :, 0, :], [pp.name], [(sv.name, 0)], eng=getattr(cfg, 'p1a_ev', 'dve'))
                    evac(sv[:, :, 1, 64:128], ppv[:, :, 1, :], [pp.name], [(sv.name, 1)], eng=getattr(cfg, 'p1a_ev', 'dve'))
                    veng = getattr(cfg, 'p1a_veng', 'pool')
                    if veng != 'none':
                        kb.op(veng, lambda e: e.tensor_copy(out=sv[:, :, 0, 64:128],
                                                            in_=val[:, 0:1].unsqueeze(1).to_broadcast([128, 4, 64])),
                              reads=[val.name], writes=[(sv.name, 2)])
                        kb.op(veng, lambda e: e.tensor_copy(out=sv[:, :, 1, 0:64],
                                                            in_=val[:, 0:1].unsqueeze(1).to_broadcast([128, 4, 64])),
                              reads=[val.name], writes=[(sv.name, 3)])
                    kb.dma('pool', scr['vaug'][t0:t0 + 128, :], sv[:].rearrange("p h t c -> p (h t c)"),
                           reads=[(sv.name, i) for i in range(4)], writes=[kb.uq()])
        kb.barrier()


DILS = (1, 4, 16)


def sl_(start, n, step):
    return slice(start, start + (n - 1) * step + 1, step)


def body_base(cfg, s):
    return sum(b for b, _ in cfg.segs[:s])


def phase1c(kb, cfg, io, scr):
    nc = kb.nc
    maxbody = max(b for b, _ in cfg.segs)
    maxT = maxbody + 2 * H
    with contextlib.ExitStack() as es:
        def sb(name, shape, dt):
            return es.enter_context(nc.sbuf_tensor(name, list(shape), dt))

        def ps(name, shape, dt=F32):
            return es.enter_context(nc.psum_tensor(name, list(shape), dt))

        biasT = sb("biasT_sb", [128, 24, 256], F32)
        qTh = [sb("qTA", [128, maxbody], BF16), sb("qTB", [128, maxbody], BF16)]
        kT = sb("kT", [128, maxT], BF16)
        acc = [sb("accA", [128, maxbody + 16], F32), sb("accB", [128, maxbody + 16], F32)]
        ntile_max = maxbody // 128 + 16
        Vbs = Rot([sb("Vb%d" % i, [128, ntile_max, 256], BF16) for i in range(2)])
        tmps = Rot([sb("atmp%d" % i, [128, 512], F32) for i in range(4)])
        pTs = Rot([sb("apT%d" % i, [128, 512], BF16) for i in range(4)])
        rdens = Rot([sb("rden%d" % i, [128, 512], F32) for i in range(2)])
        mts = Rot([sb("mt%d" % i, [128, 512], BF16) for i in range(2)])
        sps = Rot([ps("sp%d" % i, [128, 512], F32) for i in range(4)])
        pos = Rot([ps("po%d" % i, [128, 512], F32) for i in range(2)])

        kb.dma('sp', biasT[:], io['biasT'][:, :, :], writes=['biasT'])
        kb.op('pool', lambda e: e.memset(qTh[0][64:128, :], 0.0), writes=[('qT', 0)])
        kb.op('pool', lambda e: e.memset(qTh[1][0:64, :], 0.0), writes=[('qT', 1)])
        for s, (body, full) in enumerate(cfg.segs):
            T = body + 2 * H
            sl0 = cfg.slot_base[s]
            bb = body_base(cfg, s)
            for hp in range(4):
                for hh in range(2):
                    kb.dma('sp', qTh[hh][hh * 64:(hh + 1) * 64, 0:body],
                           scr['qkT'][hp * 128 + hh * 64:hp * 128 + (hh + 1) * 64, sl0 + H:sl0 + H + body],
                           reads=[('qkT', s)], writes=[('qT', hh)])
                kb.dma('sp', kT[:, 0:T], scr['qkT'][512 + hp * 128:512 + (hp + 1) * 128, sl0:sl0 + T],
                       reads=[('qkT', s)], writes=['kT'])
                kb.op('pool', lambda e: e.memset(acc[0][:, 0:body], 0.0), writes=['accA'])
                kb.op('pool', lambda e: e.memset(acc[1][:, 0:body], 0.0), writes=['accB'])
                for b, d in enumerate(DILS):
                    if b not in getattr(cfg, 'att_branches', (0, 1, 2)):
                        continue
                    Vb = Vbs.next()
                    ntr = body // (128 * d) + 1
                    nbr = body // (128 * d)
                    for r in range(d):
                        start = sl0 + r + H - 64 * d
                        for m0 in range(0, ntr, 4):
                            mc = min(4, ntr - m0)
                            src = scr['vaug'][sl_(start + m0 * 128 * d, 128 * mc, d), hp * 256:(hp + 1) * 256]
                            kb.dma('sp', Vb[:, r * ntr + m0:r * ntr + m0 + mc, :],
                                   src.rearrange("(m j) c -> j m c", j=128),
                                   reads=[('vaug', s)], writes=[(Vb.name, r, m0)], war=[Vb.name])
                    if d == 1:
                        groups = [[(0, nb0 + i) for i in range(4)] for nb0 in range(0, nbr, 4)]
                    else:
                        groups = [[(r0 + i, nb) for i in range(4)] for nb in range(nbr) for r0 in range(0, d, 4)]
                    for h in range(2):
                        hs = slice(h * 64, (h + 1) * 64)
                        accn = 'accA' if h == 0 else 'accB'
                        bh = b * 8 + hp * 2 + h
                        tasks = []
                        for gi, grp in enumerate(groups):
                            tasks.append((gi, 0, grp[0:2]))
                            tasks.append((gi, 1, grp[2:4]))
                        spt = {}
                        pot = {}

                        def emit_S(ti):
                            gi, p, qbs = tasks[ti]
                            sp_ = sps.next()
                            spt[ti] = sp_
                            spv = sp_[:].rearrange("p (q k i) -> p q k i", q=2, k=2)
                            for q, (r, nb) in enumerate(qbs):
                                qc = r + d * nb * 128
                                for kt in range(2):
                                    kc = r + H - 64 * d + d * (nb + kt) * 128
                                    kb.op('pe', lambda e: e.matmul(spv[:, q, kt, :],
                                                                   lhsT=kT[:, sl_(kc, 128, d)],
                                                                   rhs=qTh[h][:, sl_(qc, 128, d)],
                                                                   start=True, stop=True),
                                          reads=[('qT', h), 'kT'], writes=[sp_.name])

                        def emit_rest(ti):
                            gi, p, qbs = tasks[ti]
                            sp_ = spt.pop(ti)
                            tmp = tmps.next()
                            pT = pTs.next()
                            kb.op('dve', lambda e: e.scalar_tensor_tensor(
                                out=tmp[:].rearrange("p (q c) -> p q c", q=2),
                                in0=sp_[:].rearrange("p (q c) -> p q c", q=2), scalar=0.125,
                                in1=biasT[:, bh:bh + 1, :].to_broadcast([128, 2, 256]),
                                op0=ALU.mult, op1=ALU.add),
                                reads=[sp_.name, 'biasT'], writes=[tmp.name])
                            kb.op('act', lambda e: e.activation(out=pT[:], in_=tmp[:], func=AF.Exp),
                                  reads=[tmp.name], writes=[pT.name])
                            if getattr(cfg, 'att_stage', 9) < 3:
                                return
                            if p == 0:
                                pot[gi] = pos.next()
                            po = pot[gi]
                            pov = po[:].rearrange("p (q i) -> p q i", q=4)
                            pTv = pT[:].rearrange("p (q k i) -> p q k i", q=2, k=2)
                            for q, (r, nb) in enumerate(qbs):
                                for kt in range(2):
                                    kb.op('pe', lambda e: e.matmul(pov[:, p * 2 + q, :],
                                                                   lhsT=Vb[:, r * ntr + nb + kt, h * 128:(h + 1) * 128],
                                                                   rhs=pTv[:, q, kt, :],
                                                                   start=(kt == 0), stop=(kt == 1)),
                                          reads=[pT.name, Vb.name, (Vb.name, r, ((nb + kt) // 4) * 4)], writes=[po.name])
                            if p == 1:
                                grp = groups[gi]
                                r0, nb0 = grp[0]
                                a = acc[h]
                                if d == 1:
                                    c0 = nb0 * 128
                                    av = a[:, c0:c0 + 512].rearrange("p (q i) -> p q i", q=4)
                                else:
                                    c0 = r0 + d * nb0 * 128
                                    av = a[:, c0:c0 + d * 128].rearrange("p (i dd) -> p dd i", dd=d)[:, 0:4, :]
                                kb.op('dve', lambda e: e.tensor_tensor(out=av, in0=av, in1=pov[:, :, :], op=ALU.add),
                                      reads=[po.name, accn], writes=[accn])
                                pot.pop(gi)

                        stage = getattr(cfg, 'att_stage', 9)
                        if stage >= 2:
                            emit_S(0)
                            if len(tasks) > 1:
                                emit_S(1)
                            for ti in range(len(tasks)):
                                if ti + 2 < len(tasks):
                                    emit_S(ti + 2)
                                emit_rest(ti)
                for c in range(body // 512 if getattr(cfg, 'att_stage', 9) >= 1 else 0):
                    cs = slice(c * 512, (c + 1) * 512)
                    rd = rdens.next()
                    mt = mts.next()
                    kb.op('dve', lambda e: e.reciprocal(out=rd[0:64, :], in_=acc[0][64:128, cs]),
                          reads=['accA'], writes=[(rd.name, 0)])
                    kb.op('dve', lambda e: e.reciprocal(out=rd[64:128, :], in_=acc[1][0:64, cs]),
                          reads=['accB'], writes=[(rd.name, 1)])
                    kb.op('pool', lambda e: e.tensor_tensor(out=mt[0:64, :], in0=acc[0][0:64, cs], in1=rd[0:64, :],
                                                            op=ALU.mult),
                          reads=['accA', (rd.name, 0)], writes=[(mt.name, 0)])
                    kb.op('pool', lambda e: e.tensor_tensor(out=mt[64:128, :], in0=acc[1][64:128, cs],
                                                            in1=rd[64:128, :], op=ALU.mult),
                          reads=['accB', (rd.name, 1)], writes=[(mt.name, 1)])
                    kb.dma('sp', scr['mixT'][512 + hp * 128:512 + (hp + 1) * 128, bb + c * 512:bb + (c + 1) * 512],
                           mt[:], reads=[(mt.name, 0), (mt.name, 1)], writes=[kb.uq()])
        kb.barrier()


def make_cmask():
    c = np.arange(128)[:, None]
    i = np.arange(128)[None, :]
    NEG = -1.0e5
    triF = (c <= i).astype(np.float32)
    triB = (c >= i).astype(np.float32)
    mLf = np.where(i < c, 0.0, NEG)
    mLb = np.where(i > c, 0.0, NEG)
    mATf = np.where(c <= i, 0.0, NEG)
    mATb = np.where(c >= i, 0.0, NEG)
    return np.ascontiguousarray(np.stack([triF, triB, mLf, mLb, mATf, mATb], axis=1).astype(np.float32))


def phase1b(kb, cfg, io, scr):
    nc = kb.nc
    WU = 256
    maxR = max((b + 2 * WU) if f else b for b, f in cfg.segs)
    nchm = maxR // 128
    with contextlib.ExitStack() as es:
        def sb(name, shape, dt):
            return es.enter_context(nc.sbuf_tensor("b_" + name, list(shape), dt))

        banks = [es.enter_context(nc.psum_tensor("bk%d" % i, [128, 512], F32)) for i in range(8)]
        cm = sb("cm", [128, 6, 128], F32)
        identb = sb("identb", [128, 128], BF16)
        ones_f = sb("ones_f", [128, 128], F32)
        dnc = sb("dnc", [128, 16], F32)
        negA = sb("negA", [128, 8], F32)
        convw = sb("convw", [128, 12, 3], F32)
        gn = sb("gn", [128, 128], F32)
        kb.dma('sp', cm[:], io['cmask'][:, :, :], writes=['cm'])
        kb.dma('sp', identb[:], io['ident_bf'][:, :], writes=['identb'])
        kb.dma('sp', dnc[:], io['dnc'][:, :], writes=['dnc'])
        kb.dma('sp', convw[:], io['convw'][:, :, :], writes=['convw'])
        kb.dma('sp', gn[:], io['gn'][:, :], writes=['gn'])
        kb.op('pool', lambda e: e.memset(ones_f[:], 1.0), writes=['ones_f'])
        kb.op('act', lambda e: e.activation(out=negA[:], in_=dnc[:, 8:16], func=AF.Exp), reads=['dnc'], writes=['negA'])
        kb.op('dve', lambda e: e.tensor_scalar(out=negA[:], in0=negA[:], scalar1=-1.0, scalar2=None, op0=ALU.mult),
              reads=['negA'], writes=['negA'])

        def sct(name):
            return sb(name, [128, nchm, 8], F32)

        sc_t = sb("sc_t", [128, nchm, 16], F32)
        beta, gg, Gtok, Gtot, expG, expGtot, kdec, negG, coef, spx, spl, GLb = [
            sct(n) for n in ("beta", "gg", "Gtok", "Gtot", "expG", "expGtot", "kdec", "negG", "coef", "spx", "spl", "GLb")]
        CB = min(3072, maxR)
        xin = sb("xin", [128, CB + 2], F32)
        cv = sb("cv", [128, CB], F32)
        qT = sb("dqT", [128, maxR], BF16)
        kT = sb("dkT", [128, maxR], BF16)
        Ktok = sb("Ktok", [128, nchm, 128], BF16)
        Vtok = sb("Vtok", [128, nchm, 128], BF16)
        Osum = sb("Osum", [128, nchm, 128], F32)
        vTfull = Osum[:].rearrange("p c k -> p (c k)").bitcast(BF16)
        sqs = Rot([sb("sq%d" % i, [128, 512], F32) for i in range(2)])
        rss = Rot([sb("rs%d" % i, [128, 512], F32) for i in range(2)])
        S32 = [sb("S32_%d" % i, [128, 128], F32) for i in range(2)]
        Sbf = [sb("Sbf_%d" % i, [128, 128], BF16) for i in range(2)]

        def ring(name, dt, n=2, shape=(128, 128)):
            return [Rot([sb("%s_%d_%d" % (name, dr, i), list(shape), dt) for i in range(n)]) for dr in range(2)]

        G4 = (128, 4, 128)
        r_gtri = ring("gtri", F32, n=1, shape=G4)
        r_t1 = ring("t1", F32, n=1, shape=G4)
        r_t2 = ring("t2", F32, n=1, shape=G4)
        r_L = ring("Lm", BF16, n=2, shape=G4)
        r_AT = ring("ATm", BF16, n=3, shape=G4)
        r_P = ring("Pm", BF16, n=3, shape=G4)
        r_Q = ring("Qm", BF16, n=3, shape=G4)
        r_X = ring("Xm", BF16, n=3, shape=G4)
        r_TT = ring("TTm", BF16, n=3, shape=G4)
        r_Kd = ring("Kd", BF16, n=3, shape=G4)
        r_Vb = ring("Vb", BF16, n=3, shape=G4)
        r_R = ring("Rm", BF16)
        r_Vn = ring("Vn", BF16)
        r_QSs = ring("QSs", F32)
        r_tO = ring("tO", F32)
        zts = Rot([sb("zt%d" % i, [128, 128], F32) for i in range(3)])
        szs = Rot([sb("sz%d" % i, [128, 128], F32) for i in range(3)])
        gts = Rot([sb("gt%d" % i, [128, 128], F32) for i in range(3)])
        g2s = Rot([sb("g2%d" % i, [128, 128], BF16) for i in range(3)])
        gss = Rot([sb("gs%d" % i, [128, 4], F32) for i in range(3)])
        gjunk = sb("gjunk", [128, 128], F32)
        ostg = Rot([sb("ostg%d" % i, [128, 512], BF16) for i in range(2)])

        for s, (body, full) in enumerate(cfg.segs):
            R = (body + 2 * WU) if full else body
            R0 = cfg.slot_base[s] + ((H - WU) if full else H)
            nch = R // 128
            ch_b0 = (WU // 128) if full else 0
            nch_b = body // 128
            bb = body_base(cfg, s)
            NC8 = nch * 8

            def fl(t):
                return t[:, 0:nch, :].rearrange("p c k -> p (c k)")

            kb.dma('sp', sc_t[:, 0:nch, :], scr['sc'][R0:R0 + R, :].rearrange("(c p) k -> p c k", p=128),
                   reads=[('sc', s)], writes=['sc_t'])
            kb.op('act', lambda e: e.activation(out=beta[:, 0:nch, :], in_=sc_t[:, 0:nch, 0:8], func=AF.Sigmoid),
                  reads=['sc_t'], writes=['beta'])
            kb.op('dve', lambda e: e.tensor_tensor(out=spx[:, 0:nch, :], in0=sc_t[:, 0:nch, 8:16],
                                                   in1=dnc[:, 0:8].unsqueeze(1).to_broadcast([128, nch, 8]), op=ALU.add),
                  reads=['sc_t', 'dnc'], writes=['spx'])
            kb.op('dve', lambda e: e.scalar_tensor_tensor(out=spl[:, 0:nch, :], in0=spx[:, 0:nch, :], scalar=-1.0,
                                                          in1=spx[:, 0:nch, :], op0=ALU.mult, op1=ALU.max),
                  reads=['spx'], writes=['spl'])
            kb.op('act', lambda e: e.activation(out=spl[:, 0:nch, :], in_=spl[:, 0:nch, :], func=AF.Exp, scale=-1.0),
                  reads=['spl'], writes=['spl'])
            kb.op('act', lambda e: e.activation(out=spl[:, 0:nch, :], in_=spl[:, 0:nch, :], func=AF.Ln, bias=1.0),
                  reads=['spl'], writes=['spl'])
            kb.op('dve', lambda e: e.scalar_tensor_tensor(out=spx[:, 0:nch, :], in0=spx[:, 0:nch, :], scalar=0.0,
                                                          in1=spl[:, 0:nch, :], op0=ALU.max, op1=ALU.add),
                  reads=['spx', 'spl'], writes=['spx'])
            kb.op('dve', lambda e: e.tensor_tensor(out=gg[:, 0:nch, :], in0=spx[:, 0:nch, :],
                                                   in1=negA[:, 0:8].unsqueeze(1).to_broadcast([128, nch, 8]), op=ALU.mult),
                  reads=['spx', 'negA'], writes=['gg'])
            for ch in range(nch):
                kb.op('pe', lambda e: e.matmul(banks[0][:, ch * 8:ch * 8 + 4], lhsT=cm[:, 0, :], rhs=gg[:, ch, 0:4],
                                               start=True, stop=True), reads=['gg', 'cm'], writes=['bk0'])
                kb.op('pe', lambda e: e.matmul(banks[0][:, ch * 8 + 4:ch * 8 + 8], lhsT=cm[:, 1, :], rhs=gg[:, ch, 4:8],
                                               start=True, stop=True), reads=['gg', 'cm'], writes=['bk0'])
                kb.op('pe', lambda e: e.matmul(banks[1][:, ch * 8:ch * 8 + 8], lhsT=ones_f[:], rhs=gg[:, ch, :],
                                               start=True, stop=True), reads=['gg', 'ones_f'], writes=['bk1'])
            kb.op('dve', lambda e: e.tensor_copy(out=fl(Gtok), in_=banks[0][:, 0:NC8]), reads=['bk0'], writes=['Gtok'])
            kb.op('dve', lambda e: e.tensor_copy(out=fl(Gtot), in_=banks[1][:, 0:NC8]), reads=['bk1'], writes=['Gtot'])
            kb.op('act', lambda e: e.activation(out=fl(expG), in_=fl(Gtok), func=AF.Exp), reads=['Gtok'], writes=['expG'])
            kb.op('act', lambda e: e.activation(out=fl(expGtot), in_=fl(Gtot), func=AF.Exp), reads=['Gtot'],
                  writes=['expGtot'])
            kb.op('dve', lambda e: e.tensor_tensor(out=fl(kdec), in0=fl(Gtot), in1=fl(Gtok), op=ALU.subtract),
                  reads=['Gtot', 'Gtok'], writes=['kdec'])
            kb.op('act', lambda e: e.activation(out=fl(kdec), in_=fl(kdec), func=AF.Exp), reads=['kdec'], writes=['kdec'])
            kb.op('dve', lambda e: e.tensor_scalar(out=fl(negG), in0=fl(Gtok), scalar1=-1.0, scalar2=None, op0=ALU.mult),
                  reads=['Gtok'], writes=['negG'])
            kb.op('dve', lambda e: e.scalar_tensor_tensor(out=fl(coef), in0=fl(beta), scalar=-1.0, in1=fl(expG),
                                                          op0=ALU.mult, op1=ALU.mult),
                  reads=['beta', 'expG'], writes=['coef'])
            kb.op('act', lambda e: e.activation(out=fl(GLb), in_=fl(beta), func=AF.Ln), reads=['beta'], writes=['GLb'])
            kb.op('dve', lambda e: e.tensor_tensor(out=fl(GLb), in0=fl(GLb), in1=fl(Gtok), op=ALU.add),
                  reads=['GLb', 'Gtok'], writes=['GLb'])
            kb.barrier()

            for hd in range(4):
                for idx in range(3):
                    ct = idx * 4 + hd
                    row0 = idx * 512 + hd * 128
                    for c0 in range(0, R, CB):
                        cb = min(CB, R - c0)
                        lo_ = 1 if (c0 == 0 and not full) else 0
                        hi_ = 1 if (c0 + cb == R and not full) else 0
                        if lo_:
                            kb.op('pool', lambda e: e.memset(xin[:, 0:1], 0.0), writes=['xin'])
                        if hi_:
                            kb.op('pool', lambda e: e.memset(xin[:, cb + 1:cb + 2], 0.0), writes=['xin'])
                        kb.dma('sp', xin[:, lo_:cb + 2 - hi_],
                               scr['dnT'][row0:row0 + 128, R0 + c0 - 1 + lo_:R0 + c0 + cb + 1 - hi_],
                               reads=[('dnT', s)], writes=['xin'])
                        kb.op('dve', lambda e: e.tensor_scalar(out=cv[:, 0:cb], in0=xin[:, 0:cb], scalar1=convw[:, ct, 0:1],
                                                               scalar2=None, op0=ALU.mult),
                              reads=['xin', 'convw'], writes=['cv'])
                        kb.op('dve', lambda e: e.scalar_tensor_tensor(out=cv[:, 0:cb], in0=xin[:, 1:cb + 1],
                                                                      scalar=convw[:, ct, 1:2], in1=cv[:, 0:cb],
                                                                      op0=ALU.mult, op1=ALU.add),
                              reads=['xin', 'convw', 'cv'], writes=['cv'])
                        kb.op('dve', lambda e: e.scalar_tensor_tensor(out=cv[:, 0:cb], in0=xin[:, 2:cb + 2],
                                                                      scalar=convw[:, ct, 2:3], in1=cv[:, 0:cb],
                                                                      op0=ALU.mult, op1=ALU.add),
                              reads=['xin', 'convw', 'cv'], writes=['cv'])
                        if idx == 2:
                            kb.op('act', lambda e: e.activation(out=vTfull[:, c0:c0 + cb], in_=cv[:, 0:cb], func=AF.Silu),
                                  reads=['cv'], writes=['vT'])
                            continue
                        dst = qT if idx == 0 else kT
                        kb.op('act', lambda e: e.activation(out=cv[:, 0:cb], in_=cv[:, 0:cb], func=AF.Silu),
                              reads=['cv'], writes=['cv'])
                        for blk in range(cb // 512):
                            bs = slice(blk * 512, (blk + 1) * 512)
                            ds_ = slice(c0 + blk * 512, c0 + (blk + 1) * 512)
                            sq = sqs.next()
                            rs = rss.next()
                            bk = banks[2 + blk % 2]
                            kb.op('pool', lambda e: e.tensor_tensor(out=sq[:], in0=cv[:, bs], in1=cv[:, bs], op=ALU.mult),
                                  reads=['cv'], writes=[sq.name])
                            kb.op('pe', lambda e: e.matmul(bk[:, :], lhsT=ones_f[:], rhs=sq[:], start=True, stop=True),
                                  reads=[sq.name, 'ones_f'], writes=[bk.name])
                            kb.op('act', lambda e: e.activation(out=rs[:], in_=bk[:, :], func=AF.Sqrt, bias=EPS),
                                  reads=[bk.name], writes=[rs.name])
                            kb.op('dve', lambda e: e.reciprocal(out=rs[:], in_=rs[:]), reads=[rs.name], writes=[rs.name])
                            sc_ = (128.0 ** -0.5) if idx == 0 else 1.0
                            kb.op('dve', lambda e: e.scalar_tensor_tensor(out=dst[:, ds_], in0=cv[:, bs], scalar=sc_,
                                                                          in1=rs[:], op0=ALU.mult, op1=ALU.mult),
                                  reads=['cv', rs.name], writes=[dst.name])
                kb.barrier()
                for src, srck, dstt, bi in ((kT[:, :], kT.name, Ktok, 0), (vTfull, 'vT', Vtok, 1)):
                    for g0 in range(0, nch, 8):
                        gn_ = min(8, nch - g0)
                        bk = banks[bi * 2 + (g0 // 8) % 2]
                        bkb = bk[:].bitcast(BF16)
                        for c in range(gn_):
                            ch = g0 + c
                            kb.op('pe', lambda e: e.transpose(out=bkb[:, c * 128:(c + 1) * 128],
                                                              in_=src[:, ch * 128:(ch + 1) * 128], identity=identb[:]),
                                  reads=[srck, 'identb'], writes=[bk.name])
                        kb.op('dve', lambda e: e.tensor_copy(
                            out=dstt[:, g0:g0 + gn_, :].rearrange("p c k -> p (c k)"), in_=bkb[:, 0:gn_ * 128]),
                            reads=[bk.name], writes=[dstt.name])
                kb.barrier()
                kb.op('pool', lambda e: e.memset(Osum[:, 0:nch, :], 0.0), writes=['Osum'])
                for dr in range(2):
                    kb.op('pool', lambda e: e.memset(S32[dr][:], 0.0), writes=['S32_%d' % dr])
                    kb.op('pool', lambda e: e.memset(Sbf[dr][:], 0.0), writes=['Sbf_%d' % dr])
                kb.barrier()

                GB = 4
                pre = {}

                def bc_s(t, c0, col):
                    return t[:, c0:c0 + GB, col:col + 1].to_broadcast([128, GB, 128])

                def bc_m(k):
                    return cm[:, k:k + 1, :].to_broadcast([128, GB, 128])

                def v4(bank):
                    return bank[:, :].rearrange("p (g k) -> p g k", g=GB)

                def precompute(c0, dr):
                    col = dr * 4 + hd
                    MD = banks[4 * dr + 0]
                    XD = MD
                    PA = banks[4 * dr + 1]
                    QA = banks[4 * dr + 2]
                    gtri = r_gtri[dr].next(); t1 = r_t1[dr].next(); t2 = r_t2[dr].next()
                    Lm = r_L[dr].next(); ATm = r_AT[dr].next()
                    kb.op('dve', lambda e: e.tensor_tensor(out=gtri[:], in0=bc_m(dr), in1=bc_s(gg, c0, col), op=ALU.mult),
                          reads=['cm', 'gg'], writes=[gtri.name])
                    for i in range(GB):
                        kb.op('pe', lambda e: e.matmul(MD[:, i * 128:(i + 1) * 128], lhsT=ones_f[:], rhs=gtri[:, i, :],
                                                       start=True, stop=True),
                              reads=['ones_f', gtri.name], writes=[MD.name])
                    kb.op('dve', lambda e: e.tensor_tensor(out=t1[:], in0=bc_m(2 + dr), in1=v4(MD), op=ALU.subtract),
                          reads=['cm', MD.name], writes=[t1.name])
                    kb.op('dve', lambda e: e.tensor_tensor(out=t2[:], in0=bc_m(4 + dr), in1=v4(MD), op=ALU.add),
                          reads=['cm', MD.name], writes=[t2.name])
                    kb.op('dve', lambda e: e.tensor_tensor(out=t1[:], in0=t1[:], in1=bc_s(GLb, c0, col), op=ALU.add),
                          reads=[t1.name, 'GLb'], writes=[t1.name])
                    kb.op('dve', lambda e: e.tensor_tensor(out=t2[:], in0=t2[:], in1=bc_s(negG, c0, col), op=ALU.add),
                          reads=[t2.name, 'negG'], writes=[t2.name])
                    kb.op('act', lambda e: e.activation(out=t1[:], in_=t1[:], func=AF.Exp), reads=[t1.name], writes=[t1.name])
                    kb.op('act', lambda e: e.activation(out=t2[:], in_=t2[:], func=AF.Exp), reads=[t2.name], writes=[t2.name])
                    yield
                    for i in range(GB):
                        cs = slice((c0 + i) * 128, (c0 + i + 1) * 128)
                        kb.op('pe', lambda e: e.matmul(MD[:, i * 128:(i + 1) * 128], lhsT=kT[:, cs], rhs=kT[:, cs],
                                                       start=True, stop=True), reads=[kT.name], writes=[MD.name])
                    kb.op('dve', lambda e: e.tensor_tensor(out=Lm[:], in0=v4(MD), in1=t1[:], op=ALU.mult),
                          reads=[MD.name, t1.name], writes=[Lm.name])
                    for i in range(GB):
                        cs = slice((c0 + i) * 128, (c0 + i + 1) * 128)
                        kb.op('pe', lambda e: e.matmul(XD[:, i * 128:(i + 1) * 128], lhsT=kT[:, cs], rhs=qT[:, cs],
                                                       start=True, stop=True), reads=[kT.name, qT.name], writes=[XD.name])
                    kb.op('dve', lambda e: e.tensor_tensor(out=ATm[:], in0=v4(XD), in1=t2[:], op=ALU.mult),
                          reads=[XD.name, t2.name], writes=[ATm.name])
                    MDb = MD[:, :].bitcast(BF16)
                    for i in range(GB):
                        kb.op('pe', lambda e: e.transpose(out=MDb[:, i * 128:(i + 1) * 128], in_=Lm[:, i, :],
                                                          identity=identb[:]),
                              reads=[Lm.name, 'identb'], writes=[MD.name])
                    NTv = MDb[:, 0:GB * 128].rearrange("p (g k) -> p g k", g=GB)
                    P = r_P[dr].next(); X = r_X[dr].next(); Q = Lm
                    kb.op('dve', lambda e: e.tensor_copy(out=P[:], in_=NTv), reads=[MD.name], writes=[P.name])
                    kb.op('dve', lambda e: e.tensor_tensor(out=X[:], in0=identb[:].unsqueeze(1).to_broadcast([128, GB, 128]),
                                                           in1=NTv, op=ALU.subtract),
                          reads=[MD.name, 'identb'], writes=[X.name])
                    yield
                    for lvl in range(1, 7):
                        Pn = r_P[dr].next() if lvl < 6 else None
                        Qn = r_Q[dr].next()
                        if Pn is not None:
                            for i in range(GB):
                                kb.op('pe', lambda e: e.matmul(PA[:, i * 128:(i + 1) * 128], lhsT=Q[:, i, :], rhs=P[:, i, :],
                                                               start=True, stop=True),
                                      reads=[Q.name, P.name], writes=[PA.name])
                        for i in range(GB):
                            kb.op('pe', lambda e: e.matmul(QA[:, i * 128:(i + 1) * 128], lhsT=P[:, i, :], rhs=Q[:, i, :],
                                                           start=True, stop=True),
                                  reads=[Q.name, P.name], writes=[QA.name])
                        yield
                        if Pn is not None:
                            kb.op('act', lambda e: e.copy(out=Pn[:], in_=v4(PA)), reads=[PA.name], writes=[Pn.name])
                        kb.op('act', lambda e: e.copy(out=Qn[:], in_=v4(QA)), reads=[QA.name], writes=[Qn.name])
                        Xn = r_X[dr].next() if lvl < 6 else r_TT[dr].next()
                        for i in range(GB):
                            kb.op('pe', lambda e: e.matmul(XD[:, i * 128:(i + 1) * 128], lhsT=Qn[:, i, :], rhs=X[:, i, :],
                                                           start=True, stop=True),
                                  reads=[Qn.name, X.name], writes=[XD.name])
                        kb.op('dve', lambda e: e.tensor_tensor(out=Xn[:], in0=X[:], in1=v4(XD), op=ALU.add),
                              reads=[X.name, XD.name], writes=[Xn.name])
                        P, Q, X = Pn, Qn, Xn
                        yield
                    Kd = r_Kd[dr].next(); Vb = r_Vb[dr].next()
                    kb.op('pool', lambda e: e.tensor_tensor(out=Kd[:], in0=Ktok[:, c0:c0 + GB, :], in1=bc_s(kdec, c0, col),
                                                            op=ALU.mult),
                          reads=[Ktok.name, 'kdec'], writes=[Kd.name])
                    kb.op('pool', lambda e: e.tensor_tensor(out=Vb[:], in0=Vtok[:, c0:c0 + GB, :], in1=bc_s(beta, c0, col),
                                                            op=ALU.mult),
                          reads=[Vtok.name, 'beta'], writes=[Vb.name])
                    pre[(c0, dr)] = (X, ATm, Kd, Vb)
                    yield

                def chain(ch, dr):
                    col = dr * 4 + hd
                    cs = slice(ch * 128, (ch + 1) * 128)
                    B3 = banks[4 * dr + 3]
                    n3 = B3.name
                    c0 = (ch // GB) * GB
                    i = ch - c0
                    TT, ATm, Kd, Vb = pre[(c0, dr)]
                    Rm = r_R[dr].next(); Vn = r_Vn[dr].next(); QSs = r_QSs[dr].next(); tO = r_tO[dr].next()
                    sn, sbn = 'S32_%d' % dr, 'Sbf_%d' % dr
                    kb.op('pe', lambda e: e.matmul(B3[:, 0:128], lhsT=kT[:, cs], rhs=Sbf[dr][:], start=True, stop=True),
                          reads=[kT.name, sbn], writes=[n3])
                    kb.op('pe', lambda e: e.matmul(B3[:, 128:256], lhsT=qT[:, cs], rhs=Sbf[dr][:], start=True, stop=True),
                          reads=[qT.name, sbn], writes=[n3])
                    yield
                    kb.op('dve', lambda e: e.scalar_tensor_tensor(out=Rm[:], in0=B3[:, 0:128],
                                                                  scalar=coef[:, ch, col:col + 1], in1=Vb[:, i, :],
                                                                  op0=ALU.mult, op1=ALU.add),
                          reads=[n3, 'coef', Vb.name], writes=[Rm.name])
                    kb.op('dve', lambda e: e.tensor_scalar(out=QSs[:], in0=B3[:, 128:256],
                                                           scalar1=expG[:, ch, col:col + 1], scalar2=None, op0=ALU.mult),
                          reads=[n3, 'expG'], writes=[QSs.name])
                    kb.op('pe', lambda e: e.matmul(B3[:, 256:384], lhsT=TT[:, i, :], rhs=Rm[:], start=True, stop=True),
                          reads=[TT.name, Rm.name], writes=[n3])
                    yield
                    kb.op('dve', lambda e: e.tensor_copy(out=Vn[:], in_=B3[:, 256:384]), reads=[n3], writes=[Vn.name])
                    kb.op('pe', lambda e: e.matmul(B3[:, 0:128], lhsT=ATm[:, i, :], rhs=Vn[:], start=True, stop=True),
                          reads=[ATm.name, Vn.name], writes=[n3])
                    kb.op('pe', lambda e: e.matmul(B3[:, 384:512], lhsT=Kd[:, i, :], rhs=Vn[:], start=True, stop=True),
                          reads=[Kd.name, Vn.name], writes=[n3])
                    yield
                    kb.op('dve', lambda e: e.tensor_tensor(out=tO[:], in0=QSs[:], in1=B3[:, 0:128], op=ALU.add),
                          reads=[QSs.name, n3], writes=[tO.name])
                    kb.op('pool', lambda e: e.tensor_tensor(out=Osum[:, ch, :], in0=Osum[:, ch, :], in1=tO[:], op=ALU.add),
                          reads=[tO.name, ('Osum', ch)], writes=[('Osum', ch)])
                    kb.op('dve', lambda e: e.scalar_tensor_tensor(out=S32[dr][:], in0=S32[dr][:],
                                                                  scalar=expGtot[:, ch, col:col + 1], in1=B3[:, 384:512],
                                                                  op0=ALU.mult, op1=ALU.add),
                          reads=[sn, 'expGtot', n3], writes=[sn])
                    kb.op('act', lambda e: e.copy(out=Sbf[dr][:], in_=S32[dr][:]), reads=[sn], writes=[sbn])
                    yield

                nblk = nch // GB
                fblocks = [b_ * GB for b_ in range(nblk)]
                bblocks = [(nblk - 1 - b_) * GB for b_ in range(nblk)]

                def chainseq(c0, dr):
                    order_ = range(GB) if dr == 0 else range(GB - 1, -1, -1)
                    for i_ in order_:
                        for _ in chain(c0 + i_, dr):
                            yield

                gens = [precompute(fblocks[0], 0), precompute(bblocks[0], 1)]
                while gens:
                    for g_ in list(gens):
                        try:
                            next(g_)
                        except StopIteration:
                            gens.remove(g_)
                for t in range(nblk if not getattr(cfg, 'dn_nochain', False) else 0):
                    gens = [chainseq(fblocks[t], 0), chainseq(bblocks[t], 1)]
                    if t + 1 < nblk:
                        gens += [precompute(fblocks[t + 1], 0), precompute(bblocks[t + 1], 1)]
                    while gens:
                        for g_ in list(gens):
                            try:
                                next(g_)
                            except StopIteration:
                                gens.remove(g_)
                    pre.pop((fblocks[t], 0), None)
                    pre.pop((bblocks[t], 1), None)
                kb.barrier()

                for g0 in range(0, nch_b, 4):
                    bk = banks[(g0 // 4) % 2]
                    bkb = bk[:].bitcast(BF16)
                    gc_ = min(4, nch_b - g0)
                    for c in range(gc_):
                        ch = ch_b0 + g0 + c
                        zt = zts.next(); sz = szs.next(); gt = gts.next(); g2 = g2s.next(); gs = gss.next()
                        t0 = R0 + ch * 128
                        kb.dma('sp', zt[:], scr['zs'][t0:t0 + 128, hd * 128:(hd + 1) * 128],
                               reads=[('zs', s)], writes=[zt.name])
                        kb.op('act', lambda e: e.activation(out=sz[:], in_=zt[:], func=AF.Silu),
                              reads=[zt.name], writes=[sz.name])
                        kb.op('act', lambda e: e.activation(out=gjunk[:], in_=Osum[:, ch, :], func=AF.Square,
                                                            accum_out=gs[:, 0:1]),
                              reads=[('Osum', ch)], writes=['gjunk', gs.name])
                        kb.op('act', lambda e: e.activation(out=gs[:, 1:2], in_=gs[:, 0:1], func=AF.Sqrt,
                                                            scale=1.0 / 128, bias=EPS),
                              reads=[gs.name], writes=[gs.name])
                        kb.op('dve', lambda e: e.reciprocal(out=gs[:, 2:3], in_=gs[:, 1:2]), reads=[gs.name],
                              writes=[gs.name])
                        kb.op('dve', lambda e: e.scalar_tensor_tensor(out=gt[:], in0=Osum[:, ch, :], scalar=gs[:, 2:3],
                                                                      in1=gn[:], op0=ALU.mult, op1=ALU.mult),
                              reads=[('Osum', ch), gs.name, 'gn'], writes=[gt.name])
                        kb.op('pool', lambda e: e.tensor_tensor(out=g2[:], in0=gt[:], in1=sz[:], op=ALU.mult),
                              reads=[gt.name, sz.name], writes=[g2.name])
                        kb.op('pe', lambda e: e.transpose(out=bkb[:, c * 128:(c + 1) * 128], in_=g2[:], identity=identb[:]),
                              reads=[g2.name, 'identb'], writes=[bk.name])
                    og = ostg.next()
                    kb.op('dve', lambda e: e.tensor_copy(out=og[:, 0:gc_ * 128], in_=bkb[:, 0:gc_ * 128]), reads=[bk.name], writes=[og.name])
                    c0 = bb + g0 * 128
                    kb.dma('sp', scr['mixT'][hd * 128:(hd + 1) * 128, c0:c0 + gc_ * 128], og[:, 0:gc_ * 128],
                           reads=[og.name], writes=[kb.uq()])
                kb.barrier()
        kb.barrier()


def phase1d(kb, cfg, io, scr, outs):
    nc = kb.nc
    with contextlib.ExitStack() as es:
        def sb(name, shape, dt):
            return es.enter_context(nc.sbuf_tensor("d_" + name, list(shape), dt))

        banks = [es.enter_context(nc.psum_tensor("dbk%d" % i, [128, 512], F32)) for i in range(8)]
        wo = sb("wo", [128, 8, D], BF16)
        wst = [sb("wst%d" % i, [128, D], F32) for i in range(2)]
        wr = sb("wr", [128, 8, 16], F32)
        g2b = sb("g2b", [128, D], F32)
        identf = sb("identf", [128, 128], F32)
        mxs = Rot([sb("mx%d" % i, [128, 8, 512], BF16) for i in range(2)])
        xts = Rot([sb("xt%d" % i, [128, D], F32) for i in range(3)])
        x1s = Rot([sb("x1_%d" % i, [128, D], F32) for i in range(4)])
        h2fs = Rot([sb("h2f%d" % i, [128, D], F32) for i in range(5)])
        h2bs = Rot([sb("h2b%d" % i, [128, D], BF16) for i in range(2)])
        h2Ts = Rot([sb("h2T%d" % i, [128, 8, 128], F32) for i in range(2)])
        junk = sb("junk", [128, D], BF16)
        sss = Rot([sb("ss%d" % i, [128, 8], F32) for i in range(6)])
        lgs = Rot([sb("lg%d" % i, [128, 16], F32) for i in range(3)])
        afs = Rot([sb("af%d" % i, [128, 16], F32) for i in range(3)])

        kb.dma('sp', g2b[:], io['g2b'][:, :], writes=['g2b'])
        kb.dma('sp', identf[:], io['ident_f'][:, :], writes=['identf'])
        kb.dma('sp', wr[:], io['w_router'].rearrange("(k p) e -> p k e", p=128), writes=['wr'])
        for k in range(8):
            w = wst[k % 2]
            kb.dma('sp', w[:], io['w_out'][k * 128:(k + 1) * 128, :], writes=[w.name])
            kb.op('dve', lambda e: e.tensor_copy(out=wo[:, k, :], in_=w[:]), reads=[w.name], writes=[('wo', k)])
        wokeys = [('wo', k) for k in range(8)]
        pi = [0]
        for s, (body, full) in enumerate(cfg.segs):
            bb = body_base(cfg, s)
            a0 = cfg.act_base[s] + (H if full else 0)
            for g in range(body // 512):
                mx = mxs.next()
                kb.dma('sp', mx[:], scr['mixT'][:, bb + g * 512:bb + (g + 1) * 512].rearrange("(k p) t -> p k t", p=128),
                       reads=[('mixT', s)], writes=[mx.name])
                def tile_(j):
                    r0 = a0 + g * 512 + j * 128
                    o0 = bb + g * 512 + j * 128
                    xt = xts.next(); x1 = x1s.next(); h2f = h2fs.next(); h2b = h2bs.next(); h2T = h2Ts.next()
                    ss = sss.next(); lg = lgs.next(); af = afs.next()
                    kb.dma('sp', xt[:], io['xs'][r0:r0 + 128, :], writes=[xt.name])
                    for hf in range(2):
                        bk = banks[pi[0] % 4]
                        pi[0] += 1
                        for k in range(8):
                            kb.op('pe', lambda e: e.matmul(bk[:, :], lhsT=mx[:, k, j * 128:(j + 1) * 128],
                                                           rhs=wo[:, k, hf * 512:(hf + 1) * 512],
                                                           start=(k == 0), stop=(k == 7)),
                                  reads=[mx.name, wokeys[k]], writes=[bk.name])
                        kb.op('dve', lambda e: e.tensor_tensor(out=x1[:, hf * 512:(hf + 1) * 512],
                                                               in0=xt[:, hf * 512:(hf + 1) * 512], in1=bk[:, :],
                                                               op=ALU.add),
                              reads=[xt.name, bk.name], writes=[(x1.name, hf)])
                    x1k = [(x1.name, 0), (x1.name, 1)]
                    kb.dma('sp', outs['x1'][o0:o0 + 128, :], x1[:], reads=x1k, writes=['out_x1'])
                    kb.op('act', lambda e: e.activation(out=junk[:], in_=x1[:], func=AF.Square, accum_out=ss[:, 0:1]),
                          reads=x1k, writes=['junk', ss.name])
                    kb.op('act', lambda e: e.activation(out=ss[:, 1:2], in_=ss[:, 0:1], func=AF.Sqrt, scale=1.0 / D,
                                                        bias=EPS), reads=[ss.name], writes=[ss.name])
                    kb.op('dve', lambda e: e.reciprocal(out=ss[:, 2:3], in_=ss[:, 1:2]), reads=[ss.name],
                          writes=[ss.name])
                    kb.op('dve', lambda e: e.scalar_tensor_tensor(out=h2f[:], in0=x1[:], scalar=ss[:, 2:3], in1=g2b[:],
                                                                  op0=ALU.mult, op1=ALU.mult),
                          reads=x1k + [ss.name, 'g2b'], writes=[h2f.name])
                    kb.op('act', lambda e: e.copy(out=h2b[:], in_=h2f[:]), reads=[h2f.name], writes=[h2b.name])
                    kb.dma('sp', outs['h2'][o0:o0 + 128, :], h2b[:], reads=[h2b.name], writes=['out_h2'])
                    yield
                    for half in range(2):
                        bk = banks[4 + half]
                        for kk in range(4):
                            k = half * 4 + kk
                            kb.op('pe', lambda e: e.transpose(out=bk[:, kk * 128:(kk + 1) * 128],
                                                              in_=h2f[:, k * 128:(k + 1) * 128], identity=identf[:]),
                                  reads=[h2f.name, 'identf'], writes=[bk.name])
                        kb.op('dve', lambda e: e.tensor_copy(
                            out=h2T[:, half * 4:(half + 1) * 4, :].rearrange("p k t -> p (k t)"), in_=bk[:, :]),
                            reads=[bk.name], writes=[(h2T.name, half)])
                    bk = banks[6 + (pi[0] % 2)]
                    for k in range(8):
                        kb.op('pe', lambda e: e.matmul(bk[:, 0:16], lhsT=h2T[:, k, :], rhs=wr[:, k, :],
                                                       start=(k == 0), stop=(k == 7)),
                              reads=[(h2T.name, 0), (h2T.name, 1), 'wr'], writes=[bk.name])
                    kb.op('dve', lambda e: e.tensor_copy(out=lg[:], in_=bk[:, 0:16]), reads=[bk.name], writes=[lg.name])
                    kb.op('dve', lambda e: e.reduce_max(out=ss[:, 3:4], in_=lg[:], axis=AX.X), reads=[lg.name],
                          writes=[ss.name])
                    kb.op('dve', lambda e: e.tensor_scalar(out=ss[:, 4:5], in0=ss[:, 3:4], scalar1=-1.0, scalar2=None,
                                                           op0=ALU.mult), reads=[ss.name], writes=[ss.name])
                    kb.op('act', lambda e: e.activation(out=af[:], in_=lg[:], func=AF.Exp, bias=ss[:, 4:5],
                                                        accum_out=ss[:, 5:6]),
                          reads=[lg.name, ss.name], writes=[af.name, ss.name])
                    kb.op('dve', lambda e: e.reciprocal(out=ss[:, 6:7], in_=ss[:, 5:6]), reads=[ss.name],
                          writes=[ss.name])
                    kb.op('dve', lambda e: e.tensor_scalar(out=af[:], in0=af[:], scalar1=ss[:, 6:7], scalar2=None,
                                                           op0=ALU.mult), reads=[af.name, ss.name], writes=[af.name])
                    kb.dma('sp', outs['aff'][o0:o0 + 128, :], af[:], reads=[af.name], writes=['out_aff'])
                    yield
                gens_ = [tile_(j) for j in range(4)]
                for g_ in gens_:
                    next(g_)
                for g_ in gens_:
                    next(g_)
        kb.barrier()


NEXP = 16
FF = 2816
NFT = FF // 128


class Cfg2:
    def __init__(self, nb, group_tiles, nall, topk, cap, ff=2816):
        self.FF = ff
        self.NB = nb
        self.ntt = nb // 128
        self.group_tiles = group_tiles
        self.NA = nall // 128
        self.topk = topk
        self.CAP = cap
        self.CAPP = cap + 128


FULL_CFG2 = Cfg2(8192, [(0, 32), (32, 64)], 32768, 4096, 1152)


def build_program2(c2, debug=False):
    nc = bass.Bass("TRN2", target_bir_lowering=False)
    kb = KB(nc)
    io = {}

    def inp(name, shape, dt=F32):
        io[name] = nc.dram_tensor(name, list(shape), dt, kind="ExternalInput").ap()

    NB, ntt, NA, CAP, CAPP = c2.NB, c2.ntt, c2.NA, c2.CAP, c2.CAPP
    FF = c2.FF
    NFT = FF // 128
    nst = CAP // 128
    inp('x1', [NB, D])
    inp('h2p', [NB + 128, D], BF16)
    inp('aff_own', [NB, 16])
    inp('aff_all', [128, 32, NA])
    inp('w_gate', [NEXP, D, FF])
    inp('w_up', [NEXP, D, FF])
    inp('w_down', [NEXP, FF, D])
    inp('fgb', [128, D])
    inp('ustrict', [128, 128])
    inp('ident_bf', [128, 128], BF16)
    y_out = nc.dram_tensor('y', [NB, D], F32, kind="ExternalOutput").ap()
    skind = "ExternalOutput" if debug else "Internal"
    x2 = nc.dram_tensor('x2', [NB + 128, D], F32, kind=skind).ap()
    lists = [nc.dram_tensor('lists%d' % e_, [CAPP, 2], F32, kind=skind).ap() for e_ in range(NEXP)]
    thr_dbg = nc.dram_tensor('thr_dbg', [128, 32], F32, kind=skind).ap()

    with contextlib.ExitStack() as es0:
        def sb0(name, shape, dt):
            return es0.enter_context(nc.sbuf_tensor("m_" + name, list(shape), dt))

        banks = [es0.enter_context(nc.psum_tensor("mbk%d" % i, [128, 512], F32)) for i in range(8)]
        ones_f = sb0("ones_f", [128, 128], F32)
        lo = sb0("lo", [128, 32], F32)
        identb = sb0("identb", [128, 128], BF16)
        kb.op('pool', lambda e: e.memset(ones_f[:], 1.0), writes=['ones_f'])
        kb.dma('sp', identb[:], io['ident_bf'][:, :], writes=['identb'])
        rows = NB // 8
        for i in range(8):
            kb.dma('sp', x2[i * rows:(i + 1) * rows, :], io['x1'][i * rows:(i + 1) * rows, :], writes=[kb.uq()])

        with contextlib.ExitStack() as es:
            def sb(name, shape, dt):
                return es.enter_context(nc.sbuf_tensor("a_" + name, list(shape), dt))
            A = sb("A", [128, 32, NA], F32)
            cmp_ = sb("cmp", [128, 32, NA], F32)
            hi = sb("hi", [128, 32], F32)
            mid = sb("mid", [128, 32], F32)
            cnt = sb("cnt", [128, 32], F32)
            ge = sb("ge", [128, 32], F32)
            d1 = sb("d1", [128, 32], F32)
            d2 = sb("d2", [128, 32], F32)
            zrow = sb("zrow", [128, D], F32)
            kb.op('pool', lambda e: e.memset(zrow[:], 0.0), writes=['zrow'])
            kb.dma('sp', x2[NB:NB + 128, :], zrow[:], reads=['zrow'], writes=[kb.uq()])
            kb.dma('sp', A[:], io['aff_all'][:, :, :], writes=['A'])
            kb.op('pool', lambda e: e.memset(lo[:], 0.0), writes=['lo'])
            kb.op('pool', lambda e: e.memset(hi[:], 1.0), writes=['hi'])
            for it in range(32):
                kb.op('dve', lambda e: e.tensor_tensor(out=mid[:], in0=lo[:], in1=hi[:], op=ALU.add),
                      reads=['lo', 'hi'], writes=['mid'])
                kb.op('dve', lambda e: e.tensor_scalar(out=mid[:], in0=mid[:], scalar1=0.5, scalar2=None, op0=ALU.mult),
                      reads=['mid'], writes=['mid'])
                for ge_ in range(32):
                    kb.op('dve', lambda e: e.tensor_scalar(out=cmp_[:, ge_, :], in0=A[:, ge_, :],
                                                           scalar1=mid[:, ge_:ge_ + 1], scalar2=0.0, op0=ALU.is_ge,
                                                           op1=ALU.add, accum_out=cnt[:, ge_:ge_ + 1]),
                          reads=['A', 'mid'], writes=[('cmp', ge_), ('cnt', ge_)])
                kb.op('pe', lambda e: e.matmul(banks[0][:, 0:32], lhsT=ones_f[:], rhs=cnt[:], start=True, stop=True),
                      reads=[('cnt', g__) for g__ in range(32)] + ['ones_f'], writes=['mbk0'])
                kb.op('dve', lambda e: e.tensor_single_scalar(out=ge[:], in_=banks[0][:, 0:32],
                                                              scalar=float(c2.topk) - 0.5, op=ALU.is_ge),
                      reads=['mbk0'], writes=['ge'])
                kb.op('dve', lambda e: e.tensor_tensor(out=d1[:], in0=mid[:], in1=lo[:], op=ALU.subtract),
                      reads=['mid', 'lo'], writes=['d1'])
                kb.op('dve', lambda e: e.tensor_tensor(out=d1[:], in0=d1[:], in1=ge[:], op=ALU.mult),
                      reads=['d1', 'ge'], writes=['d1'])
                kb.op('dve', lambda e: e.tensor_tensor(out=d2[:], in0=hi[:], in1=mid[:], op=ALU.subtract),
                      reads=['mid', 'hi'], writes=['d2'])
                kb.op('dve', lambda e: e.tensor_tensor(out=d2[:], in0=d2[:], in1=ge[:], op=ALU.mult),
                      reads=['d2', 'ge'], writes=['d2'])
                kb.op('dve', lambda e: e.tensor_tensor(out=lo[:], in0=lo[:], in1=d1[:], op=ALU.add),
                      reads=['lo', 'd1'], writes=['lo'])
                kb.op('dve', lambda e: e.tensor_tensor(out=hi[:], in0=mid[:], in1=d2[:], op=ALU.add),
                      reads=['mid', 'd2'], writes=['hi'])
            kb.dma('sp', thr_dbg[:, :], lo[:], reads=['lo'], writes=['thr_dbg'])
            kb.barrier()

        with contextlib.ExitStack() as es:
            def sb(name, shape, dt):
                return es.enter_context(nc.sbuf_tensor("b_" + name, list(shape), dt))
            NC = ntt * 16
            affo = sb("affo", [128, ntt, 16], F32)
            sel = sb("sel", [128, ntt, 16], F32)
            within = sb("within", [128, ntt, 16], F32)
            cntb = sb("cntb", [128, ntt, 16], F32)
            incl = sb("incl", [128, ntt, 16], F32)
            zer = sb("zer", [128, ntt], F32)
            idxf = sb("idxf", [128, ntt, 16], F32)
            idxi = sb("idxi", [128, ntt, 16], I32)
            tokf = sb("tokf", [128, ntt], F32)
            src = sb("src", [128, ntt, 16, 2], F32)
            ust = sb("ust", [128, 128], F32)
            fill = sb("fill", [128, CAPP // 128, 2], F32)
            kb.dma('sp', ust[:], io['ustrict'][:, :], writes=['ust'])
            kb.dma('sp', affo[:], io['aff_own'].rearrange("(t p) e -> p t e", p=128), writes=['affo'])
            kb.op('pool', lambda e: e.memset(zer[:], 0.0), writes=['zer'])
            JF = CAPP // 128
            filli = sb("filli", [128, JF], I32)
            pio = sb("pio", [128, 1], F32)
            kb.op('pool', lambda e: e.iota(filli[:], pattern=[[1, JF]], base=0, channel_multiplier=JF), writes=['filli'])
            kb.op('pool', lambda e: e.iota(pio[:], pattern=[[0, 1]], base=CAP, channel_multiplier=1,
                                           allow_small_or_imprecise_dtypes=True), writes=['pio'])
            kb.op('dve', lambda e: e.tensor_single_scalar(out=filli[:], in_=filli[:], scalar=127, op=ALU.bitwise_and),
                  reads=['filli'], writes=['filli'])
            kb.op('dve', lambda e: e.tensor_copy(out=fill[:, :, 0], in_=filli[:]), reads=['filli'], writes=['fill'])
            kb.op('dve', lambda e: e.tensor_scalar(out=fill[:, :, 0], in0=fill[:, :, 0], scalar1=float(NB), scalar2=None,
                                                   op0=ALU.add), reads=['fill'], writes=['fill'])
            kb.op('pool', lambda e: e.memset(fill[:, :, 1:2], 0.0), reads=['fill'], writes=['fill'])
            for ex in range(NEXP):
                kb.dma('sp', lists[ex].rearrange("(p j) c -> p j c", p=128), fill[:], reads=['fill'], writes=[('lists', ex)])
            kb.op('pool', lambda e: e.iota(tokf[:], pattern=[[128, ntt]], base=0, channel_multiplier=1,
                                           allow_small_or_imprecise_dtypes=True), writes=['tokf'])
            for g, (t0, t1) in enumerate(c2.group_tiles):
                kb.op('dve', lambda e: e.tensor_tensor(
                    out=sel[:, t0:t1, :], in0=affo[:, t0:t1, :],
                    in1=lo[:, g * 16:(g + 1) * 16].unsqueeze(1).to_broadcast([128, t1 - t0, 16]), op=ALU.is_ge),
                    reads=['affo', 'lo'], writes=['sel'])

            def fl(t):
                return t[:].rearrange("p t e -> p (t e)")
            for c0 in range(0, NC, 512):
                cw = min(512, NC - c0)
                kb.op('pe', lambda e: e.matmul(banks[1][:, 0:cw], lhsT=ust[:], rhs=fl(sel)[:, c0:c0 + cw],
                                               start=True, stop=True), reads=['sel', 'ust'], writes=['mbk1'])
                kb.op('dve', lambda e: e.tensor_copy(out=fl(within)[:, c0:c0 + cw], in_=banks[1][:, 0:cw]),
                      reads=['mbk1'], writes=['within'])
                kb.op('pe', lambda e: e.matmul(banks[2][:, 0:cw], lhsT=ones_f[:], rhs=fl(sel)[:, c0:c0 + cw],
                                               start=True, stop=True), reads=['sel', 'ones_f'], writes=['mbk2'])
                kb.op('dve', lambda e: e.tensor_copy(out=fl(cntb)[:, c0:c0 + cw], in_=banks[2][:, 0:cw]),
                      reads=['mbk2'], writes=['cntb'])
            for ex in range(16):
                kb.op('dve', lambda e: e.tensor_tensor_scan(out=incl[:, :, ex], data0=cntb[:, :, ex], data1=zer[:],
                                                            initial=0.0, op0=ALU.add, op1=ALU.add),
                      reads=['cntb', 'zer'], writes=['incl'])
            kb.op('dve', lambda e: e.tensor_tensor(out=fl(idxf), in0=fl(within), in1=fl(incl), op=ALU.add),
                  reads=['within', 'incl'], writes=['idxf'])
            kb.op('dve', lambda e: e.tensor_tensor(out=fl(idxf), in0=fl(idxf), in1=fl(cntb), op=ALU.subtract),
                  reads=['idxf', 'cntb'], writes=['idxf'])
            kb.op('dve', lambda e: e.tensor_single_scalar(out=fl(incl), in_=fl(idxf), scalar=float(CAP) - 0.5, op=ALU.is_lt),
                  reads=['idxf'], writes=['incl'])
            kb.op('dve', lambda e: e.tensor_tensor(out=fl(incl), in0=fl(incl), in1=fl(sel), op=ALU.mult),
                  reads=['incl', 'sel'], writes=['incl'])
            kb.op('dve', lambda e: e.tensor_scalar(out=fl(idxf), in0=fl(idxf), scalar1=pio[:, 0:1], scalar2=None,
                                                   op0=ALU.subtract), reads=['idxf', 'pio'], writes=['idxf'])
            kb.op('dve', lambda e: e.tensor_tensor(out=fl(idxf), in0=fl(idxf), in1=fl(incl), op=ALU.mult),
                  reads=['idxf', 'incl'], writes=['idxf'])
            kb.op('dve', lambda e: e.tensor_scalar(out=fl(idxf), in0=fl(idxf), scalar1=pio[:, 0:1], scalar2=None,
                                                   op0=ALU.add), reads=['idxf', 'pio'], writes=['idxf'])
            kb.op('dve', lambda e: e.tensor_copy(out=fl(idxi), in_=fl(idxf)), reads=['idxf'], writes=['idxi'])
            kb.op('pool', lambda e: e.tensor_copy(out=src[:, :, :, 0], in_=tokf[:].unsqueeze(2).to_broadcast([128, ntt, 16])),
                  reads=['tokf'], writes=['src'])
            kb.op('pool', lambda e: e.tensor_copy(out=src[:, :, :, 1], in_=affo[:]), reads=['affo', 'src'], writes=['src'])
            for tt in range(ntt):
                for ex in range(16):
                    kb._dma_like('pool', lambda e: e.indirect_dma_start(
                        out=lists[ex][:, :], out_offset=bass.IndirectOffsetOnAxis(ap=idxi[:, tt, ex:ex + 1], axis=0),
                        in_=src[:, tt, ex, :], in_offset=None), reads=['idxi', 'src'], writes=[('lists', ex)])
            kb.barrier()

        with contextlib.ExitStack() as es:
            def sb(name, shape, dt):
                return es.enter_context(nc.sbuf_tensor("c_" + name, list(shape), dt))
            hT = sb("hT", [128, NFT, CAP], BF16)
            wd = sb("wd", [128, NFT, D], BF16)
            xeT = sb("xeT", [128, 8, CAP], BF16)
            lsts = Rot([sb("lst%d" % i, [128, nst, 2], F32) for i in range(2)])
            tokis = Rot([sb("toki%d" % i, [128, nst], I32) for i in range(2)])
            xes = Rot([sb("xe%d" % i, [128, D], BF16) for i in range(3)])
            wgst = Rot([sb("wgst%d" % i, [128, 8, 128], F32) for i in range(2)])
            wust = Rot([sb("wust%d" % i, [128, 8, 128], F32) for i in range(2)])
            wdst = Rot([sb("wdst%d" % i, [128, D], F32) for i in range(2)])
            wgbs = Rot([sb("wgb%d" % i, [128, 8, 128], BF16) for i in range(2)])
            wubs = Rot([sb("wub%d" % i, [128, 8, 128], BF16) for i in range(2)])
            sils = Rot([sb("sil%d" % i, [128, 512], F32) for i in range(2)])
            ysbs = Rot([sb("ysb%d" % i, [128, D], F32) for i in range(2)])
            kb.op('pool', lambda e: e.memset(xeT[:], 0.0), writes=['xeT'])
            sgs = [(c0, min(512, CAP - c0)) for c0 in range(0, CAP, 512)]
            for ex in range(NEXP):
                lst = lsts.next(); toki = tokis.next()
                kb.dma('sp', lst[:], lists[ex][0:CAP, :].rearrange("(j p) c -> p j c", p=128),
                       reads=[('lists', ex)], writes=[lst.name])
                kb.op('dve', lambda e: e.tensor_copy(out=toki[:], in_=lst[:, :, 0]), reads=[lst.name], writes=[toki.name])
                for j in range(nst):
                    xe = xes.next()
                    kb._dma_like('pool', lambda e: e.indirect_dma_start(
                        out=xe[:], out_offset=None, in_=io['h2p'][:, :],
                        in_offset=bass.IndirectOffsetOnAxis(ap=toki[:, j:j + 1], axis=0)),
                        reads=[toki.name], writes=[xe.name])
                    bk = banks[j % 2]
                    bkb = bk[:].bitcast(BF16)
                    for k in range(8):
                        kb.op('pe', lambda e: e.transpose(out=bkb[:, k * 128:(k + 1) * 128],
                                                          in_=xe[:, k * 128:(k + 1) * 128], identity=identb[:]),
                              reads=[xe.name, 'identb'], writes=[bk.name])
                    kb.op('dve', lambda e: e.tensor_copy(out=xeT[:, :, j * 128:(j + 1) * 128],
                                                         in_=bkb[:, 0:1024].rearrange("p (k t) -> p k t", k=8)),
                          reads=[bk.name], writes=['xeT'])
                for ft in range(NFT):
                    wg_s = wgst.next(); wu_s = wust.next(); wd_s = wdst.next(); wgb = wgbs.next(); wub = wubs.next()
                    fs = slice(ft * 128, (ft + 1) * 128)
                    for hh in range(2):
                        ks = slice(hh * 4, (hh + 1) * 4)
                        kb.dma('sp', wg_s[:, ks, :],
                               io['w_gate'][ex, hh * 512:(hh + 1) * 512, fs].rearrange("(k p) f -> p k f", p=128),
                               writes=[(wg_s.name, hh)])
                        kb.dma('sp', wu_s[:, ks, :],
                               io['w_up'][ex, hh * 512:(hh + 1) * 512, fs].rearrange("(k p) f -> p k f", p=128),
                               writes=[(wu_s.name, hh)])
                    kb.dma('sp', wd_s[:], io['w_down'][ex, fs, :], writes=[wd_s.name])
                    kb.op('pool', lambda e: e.tensor_copy(out=wgb[:], in_=wg_s[:]), reads=[(wg_s.name, 0), (wg_s.name, 1)], writes=[wgb.name])
                    kb.op('pool', lambda e: e.tensor_copy(out=wub[:], in_=wu_s[:]), reads=[(wu_s.name, 0), (wu_s.name, 1)], writes=[wub.name])
                    kb.op('pool', lambda e: e.tensor_copy(out=wd[:, ft, :], in_=wd_s[:]), reads=[wd_s.name],
                          writes=[('wd', ft)])
                    for gi, (c0, cw) in enumerate(sgs):
                        ba = banks[2 + gi % 2]
                        bbk = banks[4 + gi % 2]
                        sil = sils.next()
                        for k in range(8):
                            kb.op('pe', lambda e: e.matmul(ba[:, 0:cw], lhsT=wgb[:, k, :], rhs=xeT[:, k, c0:c0 + cw],
                                                           start=(k == 0), stop=(k == 7)),
                                  reads=[wgb.name, 'xeT'], writes=[ba.name])
                        for k in range(8):
                            kb.op('pe', lambda e: e.matmul(bbk[:, 0:cw], lhsT=wub[:, k, :], rhs=xeT[:, k, c0:c0 + cw],
                                                           start=(k == 0), stop=(k == 7)),
                                  reads=[wub.name, 'xeT'], writes=[bbk.name])
                        kb.op('act', lambda e: e.activation(out=sil[:, 0:cw], in_=ba[:, 0:cw], func=AF.Silu),
                              reads=[ba.name], writes=[sil.name])
                        kb.op('dve', lambda e: e.tensor_tensor(out=hT[:, ft, c0:c0 + cw], in0=sil[:, 0:cw],
                                                               in1=bbk[:, 0:cw], op=ALU.mult),
                              reads=[sil.name, bbk.name], writes=[('hT', ft)])
                hkeys = [('hT', ft) for ft in range(NFT)]
                wkeys = [('wd', ft) for ft in range(NFT)]
                for j in range(nst):
                    ysb = ysbs.next()
                    for hf in range(2):
                        bk = banks[6 + hf]
                        for ft in range(NFT):
                            kb.op('pe', lambda e: e.matmul(bk[:, :], lhsT=hT[:, ft, j * 128:(j + 1) * 128],
                                                           rhs=wd[:, ft, hf * 512:(hf + 1) * 512],
                                                           start=(ft == 0), stop=(ft == NFT - 1)),
                                  reads=[hkeys[ft], wkeys[ft]], writes=[bk.name])
                        kb.op('act', lambda e: e.activation(out=ysb[:, hf * 512:(hf + 1) * 512], in_=bk[:, :],
                                                            func=AF.Copy, scale=lst[:, j, 1:2]),
                              reads=[bk.name, lst.name], writes=[(ysb.name, hf)])
                    kb._dma_like('pool', lambda e: e.indirect_dma_start(
                        out=x2[:, :], out_offset=bass.IndirectOffsetOnAxis(ap=toki[:, j:j + 1], axis=0),
                        in_=ysb[:], in_offset=None, compute_op=ALU.add),
                        reads=[(ysb.name, 0), (ysb.name, 1), toki.name, 'x2'], writes=['x2'])
            kb.barrier()

        with contextlib.ExitStack() as es:
            def sb(name, shape, dt):
                return es.enter_context(nc.sbuf_tensor("f_" + name, list(shape), dt))
            fgb = sb("fgb", [128, D], F32)
            xts = Rot([sb("xt%d" % i, [128, D], F32) for i in range(3)])
            yts = Rot([sb("yt%d" % i, [128, D], F32) for i in range(3)])
            sss = Rot([sb("ss%d" % i, [128, 4], F32) for i in range(3)])
            junk = sb("junk", [128, D], BF16)
            kb.dma('sp', fgb[:], io['fgb'][:, :], writes=['fgb'])
            for tt in range(ntt):
                xt = xts.next(); yt = yts.next(); ss = sss.next()
                kb.dma('sp', xt[:], x2[tt * 128:(tt + 1) * 128, :], reads=['x2'], writes=[xt.name])
                kb.op('act', lambda e: e.activation(out=junk[:], in_=xt[:], func=AF.Square, accum_out=ss[:, 0:1]),
                      reads=[xt.name], writes=['junk', ss.name])
                kb.op('act', lambda e: e.activation(out=ss[:, 1:2], in_=ss[:, 0:1], func=AF.Sqrt, scale=1.0 / D, bias=EPS),
                      reads=[ss.name], writes=[ss.name])
                kb.op('dve', lambda e: e.reciprocal(out=ss[:, 2:3], in_=ss[:, 1:2]), reads=[ss.name], writes=[ss.name])
                kb.op('dve', lambda e: e.scalar_tensor_tensor(out=yt[:], in0=xt[:], scalar=ss[:, 2:3], in1=fgb[:],
                                                              op0=ALU.mult, op1=ALU.mult),
                      reads=[xt.name, ss.name, 'fgb'], writes=[yt.name])
                kb.dma('sp', y_out[tt * 128:(tt + 1) * 128, :], yt[:], reads=[yt.name], writes=['y_out'])
            kb.barrier()
    return nc, kb


def _t5_bucket_np(rel):
    import math
    nb = 16
    ret = np.where(rel > 0, nb, 0)
    n = np.abs(rel)
    max_exact = nb // 2
    nf = np.maximum(n, 1).astype(np.float32)
    large = max_exact + (np.log(nf / np.float32(max_exact)) / np.float32(math.log(1024 / max_exact))
                         * np.float32(nb - max_exact)).astype(np.int32)
    large = np.minimum(large, nb - 1)
    return ret + np.where(n < max_exact, n, large)


def make_biasT(rel_bias):
    j = np.arange(128)[:, None, None]
    kt = np.arange(2)[None, :, None]
    i = np.arange(128)[None, None, :]
    delta = -64 + 128 * kt + j - i
    table = np.concatenate([np.asarray(rel_bias, np.float32), np.full((1, 8), -30000.0, np.float32)], axis=0)
    out = np.zeros((128, 3, 8, 2, 128), np.float32)
    for b, d in enumerate(DILS):
        idx = _t5_bucket_np(delta * d)
        idx = np.where(np.abs(delta) <= 64, idx, 32)
        out[:, b] = np.transpose(table[idx], (0, 3, 1, 2))
    return np.ascontiguousarray(out.reshape(128, 24, 256))


def build_program(cfg, debug=False):
    nc = bass.Bass("TRN2", target_bir_lowering=False)
    kb = KB(nc)
    io = {}

    def inp(name, shape, dt=F32):
        io[name] = nc.dram_tensor(name, list(shape), dt, kind="ExternalInput").ap()

    inp('xs', [cfg.NACT, D])
    inp('valid', [cfg.NACT, 1])
    inp('w_in', [D, PW])
    inp('g1', [128, 8])
    inp('ident_bf', [128, 128], BF16)
    skind = "ExternalOutput" if debug else "Internal"
    scr = {}

    def scratch(name, shape, dt):
        scr[name] = nc.dram_tensor(name, list(shape), dt, kind=skind).ap()

    scratch('qkT', [1024, cfg.NTS], BF16)
    scratch('vaug', [cfg.NTS, 1024], BF16)
    scratch('dnT', [1536, cfg.NTS], F32)
    scratch('zs', [cfg.NTS, 512], F32)
    scratch('sc', [cfg.NTS, 16], F32)
    scratch('mixT', [1024, cfg.NBODY], BF16)
    inp('biasT', [128, 24, 256])
    if not getattr(cfg, 'skip_1a', False):
        phase1a(kb, cfg, io, scr)
    inp('cmask', [128, 6, 128])
    inp('dnc', [128, 16])
    inp('convw', [128, 12, 3])
    inp('gn', [128, 128])
    if not getattr(cfg, 'skip_1c', False):
        phase1c(kb, cfg, io, scr)
    if not getattr(cfg, 'skip_1b', False):
        phase1b(kb, cfg, io, scr)
    inp('g2b', [128, D])
    inp('ident_f', [128, 128])
    inp('w_router', [D, 16])
    inp('w_out', [D, D])
    outs = {}
    outs['x1'] = nc.dram_tensor('x1', [cfg.NBODY, D], F32, kind="ExternalOutput").ap()
    outs['h2'] = nc.dram_tensor('h2', [cfg.NBODY, D], BF16, kind="ExternalOutput").ap()
    outs['aff'] = nc.dram_tensor('aff', [cfg.NBODY, 16], F32, kind="ExternalOutput").ap()
    if not getattr(cfg, 'skip_1d', False):
        phase1d(kb, cfg, io, scr, outs)
    return nc, kb


_PROGS = {}


def _get_progs():
    if 'p1' not in _PROGS:
        _PROGS['p1'] = build_program(FULL_CFG)[0]
        _PROGS['p2'] = build_program2(FULL_CFG2)[0]
    return _PROGS['p1'], _PROGS['p2']


def kernel(x_prompt, x_sample, rel_bias, norm1_g, w_in, conv_w, a_log_fwd, dt_bias_fwd, a_log_bwd,
           dt_bias_bwd, dn_norm_g, w_out, norm2_g, w_router, w_gate, w_up, w_down, final_norm_g):
    f32 = np.float32
    x_prompt = np.asarray(x_prompt, f32)
    x_sample = np.asarray(x_sample, f32)
    nc1, nc2 = _get_progs()
    cfg = FULL_CFG
    ident_bf = np.eye(128).astype(ml_dtypes.bfloat16)
    shared1 = dict(
        w_in=np.ascontiguousarray(np.asarray(w_in, f32)[0]),
        g1=np.ascontiguousarray(np.asarray(norm1_g, f32)[0].reshape(8, 128).T),
        ident_bf=ident_bf,
        biasT=make_biasT(np.asarray(rel_bias, f32)),
        cmask=make_cmask(),
        dnc=np.ascontiguousarray(np.tile(np.concatenate([np.asarray(dt_bias_fwd, f32)[0], np.asarray(dt_bias_bwd, f32)[0],
                                                         np.asarray(a_log_fwd, f32)[0], np.asarray(a_log_bwd, f32)[0]])[None],
                                         (128, 1))),
        convw=np.ascontiguousarray(np.asarray(conv_w, f32)[0].reshape(3, 12, 128).transpose(2, 1, 0)),
        gn=np.ascontiguousarray(np.tile(np.asarray(dn_norm_g, f32)[0][None], (128, 1))),
        g2b=np.ascontiguousarray(np.tile(np.asarray(norm2_g, f32)[0][None], (128, 1))),
        ident_f=np.eye(128, dtype=f32),
        w_router=np.ascontiguousarray(np.asarray(w_router, f32)[0]),
        w_out=np.ascontiguousarray(np.asarray(w_out, f32)[0]),
    )
    SQ = 4096
    in_maps = []
    for c in range(NCORES):
        sidx, qd = c // 4, c % 4
        seg = np.zeros((SQ + 2 * H, D), f32)
        val = np.zeros((SQ + 2 * H, 1), f32)
        lo = qd * SQ - H
        hi = qd * SQ + SQ + H
        a, b = max(lo, 0), min(hi, x_sample.shape[1])
        seg[a - lo:b - lo] = x_sample[sidx, a:b]
        val[a - lo:b - lo] = 1.0
        xs = np.concatenate([x_prompt[2 * c], x_prompt[2 * c + 1], seg], axis=0)
        valid = np.concatenate([np.ones((4096, 1), f32), val], axis=0)
        m = dict(shared1)
        m['xs'] = np.ascontiguousarray(xs)
        m['valid'] = valid
        in_maps.append(m)
    r1 = run_bass_kernel_spmd(nc1, in_maps, core_ids=list(range(NCORES))).results
    aff_all = np.zeros((128, 32, 256), f32)
    for g in range(2):
        a = np.concatenate([np.asarray(r1[c]['aff'])[g * 4096:(g + 1) * 4096] for c in range(NCORES)], axis=0)
        aff_all[:, g * 16:(g + 1) * 16, :] = a.reshape(256, 128, 16).transpose(1, 2, 0)
    shared2 = dict(
        aff_all=aff_all,
        w_gate=np.ascontiguousarray(np.asarray(w_gate, f32)[0]),
        w_up=np.ascontiguousarray(np.asarray(w_up, f32)[0]),
        w_down=np.ascontiguousarray(np.asarray(w_down, f32)[0]),
        fgb=np.ascontiguousarray(np.tile(np.asarray(final_norm_g, f32)[None], (128, 1))),
        ustrict=(np.arange(128)[:, None] < np.arange(128)[None, :]).astype(f32),
        ident_bf=ident_bf,
    )
    in_maps2 = []
    for c in range(NCORES):
        m = dict(shared2)
        m['x1'] = np.asarray(r1[c]['x1'])
        m['h2p'] = np.concatenate([np.asarray(r1[c]['h2']), np.zeros((128, D), ml_dtypes.bfloat16)], axis=0)
        m['aff_own'] = np.asarray(r1[c]['aff'])
        in_maps2.append(m)
    r2 = run_bass_kernel_spmd(nc2, in_maps2, core_ids=list(range(NCORES))).results
    y_prompt = np.zeros(x_prompt.shape, f32)
    y_sample = np.zeros(x_sample.shape, f32)
    for c in range(NCORES):
        y = np.asarray(r2[c]['y'])
        y_prompt[2 * c] = y[0:2048]
        y_prompt[2 * c + 1] = y[2048:4096]
        y_sample[c // 4, (c % 4) * SQ:(c % 4 + 1) * SQ] = y[4096:8192]
    return (y_prompt, y_sample)
```
